# Optimizing a Trainium2 kernel written in Bass

```python
import math
import jax, jax.numpy as jnp
from jax import lax
import numpy as np

D_MODEL = 1024
BATCH = 4
SEQ = 8192
DEPTH = 2

N_MIXERS = 2
N_MAMBA = (DEPTH + 1) // 2
N_MOBA = DEPTH // 2

SSM_EXPAND = 2
SSM_D_INNER = SSM_EXPAND * D_MODEL
SSM_HEAD_DIM = 64
SSM_HEADS = SSM_D_INNER // SSM_HEAD_DIM
SSM_GROUPS = 8
SSM_HEADS_PER_GROUP = SSM_HEADS // SSM_GROUPS
SSM_STATE = 128
SSM_CONV = 4
SSM_CHUNK = 256
SSM_CONV_DIM = SSM_D_INNER + 2 * SSM_GROUPS * SSM_STATE
SSM_IN_DIM = SSM_D_INNER + SSM_CONV_DIM + SSM_HEADS

ATTN_HEAD_DIM = 64
ATTN_HEADS = D_MODEL // ATTN_HEAD_DIM
MOBA_BLOCK = 256
MOBA_TOPK = 3
MOBA_Q_CHUNK = 16
ALIBI_MAX_BIAS = 8.0

D_FF = 2816
RMS_EPS = 1e-6

kernel_name = "hybrid_mamba2_moba_macaron_adaln"


def rmsnorm(x, g):
    xf = x.astype(jnp.float32)
    y = xf * lax.rsqrt(jnp.mean(xf * xf, axis=-1, keepdims=True) + RMS_EPS)
    return (y * g.astype(jnp.float32)).astype(x.dtype)


def modulate(x, g, c_act, w, b):
    m = c_act @ w + b
    shift, scale, gate = jnp.split(m, 3, axis=-1)
    h = rmsnorm(x, g) * (1.0 + scale[:, None, :]) + shift[:, None, :]
    return h, gate[:, None, :]


def swiglu(h, w_gate, w_up, w_down):
    return (jax.nn.silu(h @ w_gate) * (h @ w_up)) @ w_down


def ssd_chunked_scan(x, dt, a, bm, cm):
    bsz, L = x.shape[0], x.shape[1]
    n_chunks = -(-L // SSM_CHUNK)
    pad = n_chunks * SSM_CHUNK - L

    def to_chunks(t):
        t = jnp.pad(t.astype(jnp.float32), [(0, 0), (0, pad)] + [(0, 0)] * (t.ndim - 2))
        t = t.reshape((bsz, n_chunks, SSM_CHUNK) + t.shape[2:])
        return jnp.moveaxis(t, 1, 0)

    xc, dtc, bc, cc = to_chunks(x), to_chunks(dt), to_chunks(bm), to_chunks(cm)
    causal = jnp.tril(jnp.ones((SSM_CHUNK, SSM_CHUNK), dtype=bool))[None, :, :, None, None]

    def step(state, inp):
        x_k, dt_k, b_k, c_k = inp
        acum = jnp.cumsum(dt_k * a, axis=1)
        seg = acum[:, :, None] - acum[:, None, :]
        decay = jnp.exp(jnp.where(causal, seg, -jnp.inf))
        cb = jnp.einsum('btgn,bsgn->btsg', c_k, b_k)
        w = cb[..., None] * decay * dt_k[:, None]
        y_intra = jnp.einsum('btsgr,bsgrp->btgrp', w, x_k)
        y_inter = jnp.einsum('btgn,bgrpn->btgrp', c_k, state) * jnp.exp(acum)[..., None]
        to_end = jnp.exp(acum[:, -1:] - acum) * dt_k
        new_state = (state * jnp.exp(acum[:, -1])[..., None, None]
                     + jnp.einsum('bsgn,bsgr,bsgrp->bgrpn', b_k, to_end, x_k))
        return new_state, y_intra + y_inter

    state0 = jnp.zeros((bsz, SSM_GROUPS, SSM_HEADS_PER_GROUP, SSM_HEAD_DIM, SSM_STATE), jnp.float32)
    _, y = lax.scan(step, state0, (xc, dtc, bc, cc))
    y = jnp.moveaxis(y, 0, 1).reshape((bsz, n_chunks * SSM_CHUNK) + y.shape[3:])[:, :L]
    return y.astype(x.dtype)


def mamba2_mixer(h, w_in, conv_w, conv_b, dt_bias, a_log, d_skip, norm_w, w_out):
    bsz, L, _ = h.shape
    G, R, P, N = SSM_GROUPS, SSM_HEADS_PER_GROUP, SSM_HEAD_DIM, SSM_STATE
    proj = h @ w_in
    z, xbc, dt = jnp.split(proj, [SSM_D_INNER, SSM_D_INNER + SSM_CONV_DIM], axis=-1)
    xbc = lax.conv_general_dilated(
        xbc, conv_w, window_strides=(1,), padding=[(SSM_CONV - 1, 0)],
        dimension_numbers=('NWC', 'WIO', 'NWC'), feature_group_count=SSM_CONV_DIM) + conv_b
    xbc = jax.nn.silu(xbc)
    xs, bm, cm = jnp.split(xbc, [SSM_D_INNER, SSM_D_INNER + G * N], axis=-1)
    dt = jax.nn.softplus(dt.astype(jnp.float32) + dt_bias.astype(jnp.float32))
    a = -jnp.exp(a_log.astype(jnp.float32))
    xs = xs.reshape(bsz, L, G, R, P)
    y = ssd_chunked_scan(xs, dt.reshape(bsz, L, G, R), a.reshape(G, R),
                         bm.reshape(bsz, L, G, N), cm.reshape(bsz, L, G, N))
    y = y + d_skip.reshape(G, R)[..., None] * xs
    y = y.reshape(bsz, L, SSM_D_INNER)
    y = rmsnorm(y * jax.nn.silu(z), norm_w)
    return y @ w_out


def moba_mixer(h, w_qkv, w_out):
    bsz, L, _ = h.shape
    H, dh, BLK, QC = ATTN_HEADS, ATTN_HEAD_DIM, MOBA_BLOCK, MOBA_Q_CHUNK
    qkv = (h @ w_qkv).reshape(bsz, L, 3, H, dh)
    q = jnp.transpose(qkv[:, :, 0], (0, 2, 1, 3))
    k = jnp.transpose(qkv[:, :, 1], (0, 2, 1, 3))
    v = jnp.transpose(qkv[:, :, 2], (0, 2, 1, 3))
    n_blocks = -(-L // BLK)
    K = max(1, min(MOBA_TOPK, n_blocks))
    pad = n_blocks * BLK - L
    k = jnp.pad(k, [(0, 0), (0, 0), (0, pad), (0, 0)])
    v = jnp.pad(v, [(0, 0), (0, 0), (0, pad), (0, 0)])
    kb = k.reshape(bsz, H, n_blocks, BLK, dh)
    vb = v.reshape(bsz, H, n_blocks, BLK, dh)
    kmean = jnp.mean(kb.astype(jnp.float32), axis=3)
    scale = dh ** -0.5
    slopes = jnp.exp2(-ALIBI_MAX_BIAS * (jnp.arange(H, dtype=jnp.float32) + 1.0) / H)
    bi = jnp.arange(bsz)[:, None, None, None]
    hi = jnp.arange(H)[None, :, None, None]
    blk_ids = jnp.arange(n_blocks)
    in_blk = jnp.arange(BLK)

    def chunk(ci):
        q0 = ci * QC
        own = q0 // BLK
        qc = lax.dynamic_slice_in_dim(q, q0, QC, axis=2)
        qpos = q0 + jnp.arange(QC)
        gate = jnp.einsum('bhqd,bhnd->bhqn', qc.astype(jnp.float32), kmean)
        gate = jnp.where((blk_ids < own)[None, None, None, :], gate, -jnp.inf)
        _, sel = lax.top_k(gate, K)
        sel_valid = jnp.arange(K) < own
        k_sel = kb[bi, hi, sel]
        v_sel = vb[bi, hi, sel]
        kpos_sel = sel[..., None] * BLK + in_blk
        dist_sel = (qpos[:, None, None] - kpos_sel).astype(jnp.float32)
        s_sel = (jnp.einsum('bhqd,bhqkjd->bhqkj', qc, k_sel).astype(jnp.float32) * scale
                 - slopes[:, None, None, None] * dist_sel)
        s_sel = jnp.where(sel_valid[:, None], s_sel, -jnp.inf)
        k_own = lax.dynamic_slice_in_dim(k, own * BLK, BLK, axis=2)
        v_own = lax.dynamic_slice_in_dim(v, own * BLK, BLK, axis=2)
        kpos_own = own * BLK + in_blk
        dist_own = (qpos[:, None] - kpos_own[None, :]).astype(jnp.float32)
        s_own = (jnp.einsum('bhqd,bhjd->bhqj', qc, k_own).astype(jnp.float32) * scale
                 - slopes[:, None, None] * dist_own)
        s_own = jnp.where(dist_own >= 0.0, s_own, -jnp.inf)
        s = jnp.concatenate([s_own, s_sel.reshape(bsz, H, QC, K * BLK)], axis=-1)
        p = jax.nn.softmax(s, axis=-1).astype(v.dtype)
        p_own = p[..., :BLK]
        p_sel = p[..., BLK:].reshape(bsz, H, QC, K, BLK)
        return (jnp.einsum('bhqj,bhjd->bhqd', p_own, v_own)
                + jnp.einsum('bhqkj,bhqkjd->bhqd', p_sel, v_sel))

    out = lax.map(chunk, jnp.arange(L // QC))
    out = jnp.transpose(out, (1, 0, 3, 2, 4)).reshape(bsz, L, H * dh)
    return out @ w_out


def setup_inputs(seed: int = 0) -> dict:
    key = jax.random.key(seed)
    ks = jax.random.split(key, 24)
    f32 = jnp.float32

    def nrm(k, shape, s):
        return jax.random.normal(k, shape, f32) * s

    x = nrm(ks[0], (BATCH, SEQ, D_MODEL), 1.0)
    c = nrm(ks[1], (BATCH, D_MODEL), 1.0)
    norm_g = 1.0 + nrm(ks[2], (DEPTH, 3, D_MODEL), 0.05)
    mod_w = nrm(ks[3], (DEPTH, 3, D_MODEL, 3 * D_MODEL), 0.5 * D_MODEL ** -0.5)
    mod_b = nrm(ks[4], (DEPTH, 3, 3 * D_MODEL), 0.02)
    ffn_w_gate = nrm(ks[5], (DEPTH, 2, D_MODEL, D_FF), D_MODEL ** -0.5)
    ffn_w_up = nrm(ks[6], (DEPTH, 2, D_MODEL, D_FF), D_MODEL ** -0.5)
    ffn_w_down = nrm(ks[7], (DEPTH, 2, D_FF, D_MODEL), D_FF ** -0.5)
    ssm_w_in = nrm(ks[8], (N_MAMBA, D_MODEL, SSM_IN_DIM), D_MODEL ** -0.5)
    ssm_conv_w = nrm(ks[9], (N_MAMBA, SSM_CONV, 1, SSM_CONV_DIM), SSM_CONV ** -0.5)
    ssm_conv_b = nrm(ks[10], (N_MAMBA, SSM_CONV_DIM), 0.02)
    dt0 = jnp.exp(jax.random.uniform(ks[11], (N_MAMBA, SSM_HEADS), f32,
                                     math.log(1e-3), math.log(1e-1)))
    ssm_dt_bias = dt0 + jnp.log(-jnp.expm1(-dt0))
    ssm_a_log = jnp.log(jax.random.uniform(ks[12], (N_MAMBA, SSM_HEADS), f32, 1.0, 16.0))
    ssm_d = 1.0 + nrm(ks[13], (N_MAMBA, SSM_HEADS), 0.05)
    ssm_norm_w = 1.0 + nrm(ks[14], (N_MAMBA, SSM_D_INNER), 0.05)
    ssm_w_out = nrm(ks[15], (N_MAMBA, SSM_D_INNER, D_MODEL), SSM_D_INNER ** -0.5)
    attn_w_qkv = nrm(ks[16], (N_MOBA, D_MODEL, 3 * D_MODEL), D_MODEL ** -0.5)
    attn_w_out = nrm(ks[17], (N_MOBA, D_MODEL, D_MODEL), D_MODEL ** -0.5)
    final_norm_g = 1.0 + nrm(ks[18], (D_MODEL,), 0.05)
    return {"x": x, "c": c, "norm_g": norm_g, "mod_w": mod_w, "mod_b": mod_b,
            "ffn_w_gate": ffn_w_gate, "ffn_w_up": ffn_w_up, "ffn_w_down": ffn_w_down,
            "ssm_w_in": ssm_w_in, "ssm_conv_w": ssm_conv_w, "ssm_conv_b": ssm_conv_b,
            "ssm_dt_bias": ssm_dt_bias, "ssm_a_log": ssm_a_log, "ssm_d": ssm_d,
            "ssm_norm_w": ssm_norm_w, "ssm_w_out": ssm_w_out,
            "attn_w_qkv": attn_w_qkv, "attn_w_out": attn_w_out,
            "final_norm_g": final_norm_g}


def reference(x, c, norm_g, mod_w, mod_b, ffn_w_gate, ffn_w_up, ffn_w_down,
              ssm_w_in, ssm_conv_w, ssm_conv_b, ssm_dt_bias, ssm_a_log, ssm_d,
              ssm_norm_w, ssm_w_out, attn_w_qkv, attn_w_out, final_norm_g):
    c_act = jax.nn.silu(c)
    for i in range(DEPTH):
        h, g = modulate(x, norm_g[i, 0], c_act, mod_w[i, 0], mod_b[i, 0])
        x = x + 0.5 * g * swiglu(h, ffn_w_gate[i, 0], ffn_w_up[i, 0], ffn_w_down[i, 0])
        h, g = modulate(x, norm_g[i, 1], c_act, mod_w[i, 1], mod_b[i, 1])
        j = i // N_MIXERS
        if i % N_MIXERS == 0:
            y = mamba2_mixer(h, ssm_w_in[j], ssm_conv_w[j], ssm_conv_b[j], ssm_dt_bias[j],
                             ssm_a_log[j], ssm_d[j], ssm_norm_w[j], ssm_w_out[j])
        else:
            y = moba_mixer(h, attn_w_qkv[j], attn_w_out[j])
        x = x + g * y
        h, g = modulate(x, norm_g[i, 2], c_act, mod_w[i, 2], mod_b[i, 2])
        x = x + 0.5 * g * swiglu(h, ffn_w_gate[i, 1], ffn_w_up[i, 1], ffn_w_down[i, 1])
    return rmsnorm(x, final_norm_g)
```

```python
import bisect
from contextlib import ExitStack
import numpy as np
import concourse.bass as bass
import concourse.mybir as mybir
from concourse.bass_utils import run_bass_kernel_spmd

F32, BF16 = mybir.dt.float32, mybir.dt.bfloat16
AF = mybir.ActivationFunctionType
ALU = mybir.AluOpType
AX = mybir.AxisListType

D = 1024
DFF = 2816
NFC = DFF // 128
EPS = 1e-6
ENG = ['pe', 'act', 'dve', 'pool', 'sp']


class Buf:
    __slots__ = ('w', 'r', 'name', 'dsem')

    def __init__(self, name=''):
        self.w = None
        self.r = {}
        self.name = name
        self.dsem = None


class DSem:
    __slots__ = ('h', 'n')

    def __init__(self, h):
        self.h = h
        self.n = 0


class Ctx:
    def __init__(self, nc):
        self.nc = nc
        self.eng = {'pe': nc.tensor, 'act': nc.scalar, 'dve': nc.vector, 'pool': nc.gpsimd, 'sp': nc.sync}
        self.inst = {e: [] for e in ENG}
        self.ms_seq = {e: [] for e in ENG}
        self.sem = {}
        self.nsem = 0
        for e in ('pe', 'act', 'dve'):
            self.sem[e] = nc.alloc_semaphore(f'es_{e}_{self.nsem}')
        self.known = {e: {} for e in ENG}
        self.base = {e: 0 for e in ENG}
        self.dpool = []
        self.dall = []
        self.ndma = 0

    def _resolve(self, tok):
        if tok[0] == 'd':
            return tok[1].h, id(tok[1]), tok[2]
        _, e, seq = tok
        seqs = self.ms_seq[e]
        i = bisect.bisect_left(seqs, seq)
        if i < len(seqs):
            return self.sem[e], ('e', e), i + 1
        ins = self.inst[e][seq - 1]
        ins.then_inc(self.sem[e], 1)
        seqs.append(seq)
        return self.sem[e], ('e', e), len(seqs)

    def _wait(self, e, tok):
        if tok is None:
            return
        if tok[0] == 'e' and tok[2] <= self.base[tok[1]]:
            return
        if tok[0] == 'e' and tok[1] == e:
            if e == 'pe':
                return
        h, key, val = self._resolve(tok)
        if self.known[e].get(key, 0) >= val:
            return
        self.known[e][key] = val
        self.eng[e].wait_ge(h, val)

    def _hazards(self, e, reads, writes):
        for b in reads:
            self._wait(e, b.w)
        for b in writes:
            self._wait(e, b.w)
            for t in b.r.values():
                self._wait(e, t)

    def op(self, e, fn, reads=(), writes=()):
        self._hazards(e, reads, writes)
        ins = fn()
        self.inst[e].append(ins)
        tok = ('e', e, len(self.inst[e]))
        for b in reads:
            b.r[e] = tok
        for b in writes:
            b.w = tok
            b.r = {}
        return ins

    def _get_dsem(self, b):
        if b.dsem is None:
            if self.dpool:
                b.dsem = self.dpool.pop()
            else:
                b.dsem = DSem(self.nc.alloc_semaphore(f'ds_{len(self.dall)}'))
                self.dall.append(b.dsem)
        return b.dsem

    def dma(self, q, out, in_, reads=(), writes=(), **kw):
        self._hazards(q, reads, writes)
        ins = self.eng[q].dma_start(out=out, in_=in_, **kw)
        b = writes[0] if writes else reads[0]
        ds = self._get_dsem(b)
        ins.then_inc(ds.h, 16)
        ds.n += 16
        tok = ('d', ds, ds.n)
        self.ndma += 1
        for b in reads:
            b.r[('d', id(ds))] = tok
        for b in writes:
            b.w = tok
            b.r = {}
        return ins

    def barrier(self, bufs_to_release=()):
        for e in ENG:
            for o in ('pe', 'act', 'dve'):
                if o != e and self.inst[o]:
                    self._wait(e, ('e', o, len(self.inst[o])))
            for ds in self.dall:
                if ds.n:
                    self._wait(e, ('d', ds, ds.n))
        for b in bufs_to_release:
            if b.dsem is not None:
                self.dpool.append(b.dsem)
                b.dsem = None
        self.nsem += 1
        for e in ('pe', 'act', 'dve'):
            self.base[e] = len(self.inst[e])
            if self.ms_seq[e]:
                self.sem[e] = self.nc.alloc_semaphore(f'es_{e}_{self.nsem}')
                self.ms_seq[e] = []
            for k in ENG:
                self.known[k].pop(('e', e), None)


class PView:
    def __init__(self, t, shape):
        self.t = t
        self.shape = shape
        n = 1
        for d in shape[1:]:
            n *= d
        base = t[0:shape[0], 0:n]
        if len(shape) == 3:
            base = base.rearrange("p (a b) -> p a b", a=shape[1])
        self.base = base

    def __getitem__(self, idx):
        return self.base[idx]


class Stage:
    def __init__(self, cx, name):
        self.cx = cx
        self.nc = cx.nc
        self.name = name
        self.es = ExitStack()
        self.bufs = []
        self.n = 0

    def sb(self, shape, dt, name=None):
        self.n += 1
        return self.es.enter_context(self.nc.sbuf_tensor(f'{self.name}_{name or "t"}{self.n}', list(shape), dt))

    def ps(self, shape, dt=F32, name=None):
        self.n += 1
        full = 512 if dt == F32 else 1024
        t = self.es.enter_context(self.nc.psum_tensor(f'{self.name}_{name or "p"}{self.n}', [128, full], dt))
        return PView(t, list(shape))

    def buf(self, name=''):
        b = Buf(name)
        self.bufs.append(b)
        return b

    def close(self):
        self.cx.barrier(self.bufs)
        self.es.close()


def load_w_bf16(cx, st, w_d, K, Fdim, f0=0, fw=None, name='w', stg=None):
    nc = cx.nc
    fw = fw or Fdim
    kc_n = K // 128
    t = st.sb([128, kc_n, fw], BF16, name)
    if stg is None:
        stg = (st.sb([128, 2, fw], F32, name + 'stg'), [st.buf(name + 's0'), st.buf(name + 's1')])
    stg, stgb = stg
    bufs = []
    for kc in range(kc_n):
        b = st.buf(f'{name}{kc}')
        j = kc % 2
        cx.dma('sp', stg[:, j, 0:fw], w_d[kc * 128:(kc + 1) * 128, f0:f0 + fw], writes=[stgb[j]])
        for a0 in range(0, fw, 2048):
            a1 = min(fw, a0 + 2048)
            cx.op('act', lambda kc=kc, j=j, a0=a0, a1=a1: nc.scalar.copy(out=t[:, kc, a0:a1], in_=stg[:, j, a0:a1]),
                  reads=[stgb[j]], writes=[b])
        bufs.append(b)
    return t, bufs


def load_cast_small(cx, st, dst, src_d, shape, b, name):
    nc = cx.nc
    tmp = st.sb(shape, F32, name + 'f')
    tb = st.buf(name + 'f')
    idx = tuple(slice(None) for _ in shape)
    cx.dma('sp', tmp[idx], src_d, writes=[tb])
    cx.op('dve', lambda: nc.vector.tensor_copy(out=dst[idx], in_=tmp[idx]), reads=[tb], writes=[b])


def rms_mod(cx, st, R, xt, xb, A, SH, T, nchunk=8, dim=D):
    nc = cx.nc
    sq, sqb, pss, pssb, rstd, rstdb, tmp, tmpb, h, hb = (R[k] for k in
                                                         ('sq', 'sqb', 'pss', 'pssb', 'rstd', 'rstdb', 'tmp', 'tmpb', 'h', 'hb'))
    ones = R['ones_bf']
    cx.op('act', lambda: nc.scalar.activation(out=sq[:, :, :], in_=xt, func=AF.Square), reads=[xb], writes=[sqb])
    for c in range(nchunk):
        cx.op('pe', lambda c=c: nc.tensor.matmul(pss[:, :], ones[:, :], sq[:, c, :], start=(c == 0), stop=(c == nchunk - 1)),
              reads=[sqb], writes=[pssb])
    cx.op('act', lambda: nc.scalar.activation(out=rstd[:, :], in_=pss[:, :], func=AF.Sqrt, bias=R['epsc'][:, 0:1],
                                              scale=1.0), reads=[pssb], writes=[rstdb])
    cx.op('dve', lambda: nc.vector.reciprocal(out=rstd[:, :], in_=rstd[:, :]), reads=[rstdb], writes=[rstdb])
    for c in range(nchunk):
        j = c % 2
        cx.op('dve', lambda c=c, j=j: nc.vector.scalar_tensor_tensor(out=tmp[:, j, :], in0=xt[:, c, :], scalar=float(dim) ** 0.5,
                                                                     in1=rstd[:, :], op0=ALU.mult, op1=ALU.mult),
              reads=[xb, rstdb], writes=[tmpb[j]])
        cx.op('act', lambda c=c, j=j: nc.scalar.activation(out=h[:, c, :], in_=tmp[:, j, :], func=AF.Identity,
                                                           bias=SH[:, c:c + 1], scale=A[:, c:c + 1]),
              reads=[tmpb[j]], writes=[hb])


def norm_res(st, T, nchunk=8):
    R = {}
    R['sq'] = st.sb([128, nchunk, T], BF16, 'sq'); R['sqb'] = st.buf('sq')
    R['pss'] = st.ps([128, T], F32, 'pss'); R['pssb'] = st.buf('pss')
    R['rstd'] = st.sb([128, T], F32, 'rstd'); R['rstdb'] = st.buf('rstd')
    R['tmp'] = st.sb([128, 2, T], F32, 'tmp'); R['tmpb'] = [st.buf('tmp0'), st.buf('tmp1')]
    R['h'] = st.sb([128, nchunk, T], BF16, 'h'); R['hb'] = st.buf('h')
    return R


def mod_stage(cx, P, G):
    nc = cx.nc
    st = Stage(cx, 'mod')
    ccol = st.sb([128, 8], F32, 'ccol'); ccb = st.buf('cc')
    cact = st.sb([128, 8], F32, 'cact'); cab = st.buf('ca')
    sig = st.sb([128, 8], F32, 'sig')
    mb = st.sb([128, 6, 24], F32, 'mb'); mbb = st.buf('mb')
    ng = st.sb([128, 6, 8], F32, 'ng'); ngb = st.buf('ng')
    m = st.sb([128, 6, 24], F32, 'm'); mbuf = st.buf('m')
    FW = 1536
    wt = [st.sb([128, 8, FW], F32, f'w{i}') for i in range(2)]
    wtb = [[st.buf(f'w{i}_{kc}') for kc in range(8)] for i in range(2)]
    ps = [st.ps([128, 12], F32, f'ps{i}') for i in range(2)]
    psb = [st.buf(f'ps{i}') for i in range(2)]
    cx.dma('sp', ccol[:, :], P['ccol'], writes=[ccb])
    cx.dma('sp', mb[:, :, :], P['modb'], writes=[mbb])
    cx.dma('sp', ng[:, :, :], P['ng'], writes=[ngb])
    cx.op('act', lambda: nc.scalar.activation(out=sig[:, :], in_=ccol[:, :], func=AF.Sigmoid), reads=[ccb], writes=[cab])
    cx.op('dve', lambda: nc.vector.tensor_tensor(out=cact[:, :], in0=sig[:, :], in1=ccol[:, :], op=ALU.mult),
          reads=[cab, ccb], writes=[cab])
    it = 0
    for ij in range(6):
        for half in range(2):
            s = it % 2
            it += 1
            for kc in range(8):
                cx.dma('sp', wt[s][:, kc, :], P['modw'][ij, kc * 128:(kc + 1) * 128, half * FW:(half + 1) * FW],
                       writes=[wtb[s][kc]])
            for fc in range(12):
                for kc in range(8):
                    cx.op('pe', lambda s=s, fc=fc, kc=kc: nc.tensor.matmul(
                        ps[s][:, fc:fc + 1], wt[s][:, kc, fc * 128:(fc + 1) * 128], cact[:, kc:kc + 1],
                        start=(kc == 0), stop=(kc == 7)), reads=[wtb[s][kc], cab], writes=[psb[s]])
            cx.op('dve', lambda s=s, ij=ij, half=half: nc.vector.tensor_tensor(
                out=m[:, ij, half * 12:(half + 1) * 12], in0=ps[s][:, :], in1=mb[:, ij, half * 12:(half + 1) * 12],
                op=ALU.add), reads=[psb[s], mbb], writes=[mbuf])
    A, SH, GT, HG = G['A'], G['SH'], G['GT'], G['HG']
    gb = G['gb']
    cx.op('dve', lambda: nc.vector.scalar_tensor_tensor(out=A[:, :, :], in0=m[:, :, 8:16], scalar=1.0, in1=ng[:, :, :],
                                                       op0=ALU.add, op1=ALU.mult), reads=[mbuf, ngb], writes=[gb])
    cx.op('dve', lambda: nc.vector.tensor_copy(out=SH[:, :, :], in_=m[:, :, 0:8]), reads=[mbuf], writes=[gb])
    cx.op('dve', lambda: nc.vector.tensor_copy(out=GT[:, :, :], in_=m[:, :, 16:24]), reads=[mbuf], writes=[gb])
    cx.op('dve', lambda: nc.vector.tensor_scalar(out=HG[:, :, :], in0=m[:, :, 16:24], scalar1=0.5, scalar2=0.0,
                                                 op0=ALU.mult, op1=ALU.add), reads=[mbuf], writes=[gb])
    st.close()


def ffn_stage(cx, P, G, x_in, x_out, fi, ij, L, T=256, final=False):
    nc = cx.nc
    st = Stage(cx, f'ffn{fi}')
    stg = (st.sb([128, 2, DFF], F32, 'wstg'), [st.buf('ws0'), st.buf('ws1')])
    wg, wgb = load_w_bf16(cx, st, P['wg'][fi], D, DFF, name='wg', stg=stg)
    wu, wub = load_w_bf16(cx, st, P['wu'][fi], D, DFF, name='wu', stg=stg)
    wd, wdb = load_w_bf16(cx, st, P['wd'][fi], DFF, D, name='wd', stg=stg)
    R = norm_res(st, T)
    R['ones_bf'] = G['ones_bf']; R['epsc'] = G['epsc']
    xs = [st.sb([128, 8, T], F32, f'x{i}') for i in range(2)]
    xb = [st.buf(f'x{i}') for i in range(2)]
    act = st.sb([128, NFC, T], BF16, 'act'); actb = [st.buf(f'act{f}') for f in range(NFC)]
    sg = st.sb([128, 2, T], F32, 'sg'); sgb = [st.buf('sg0'), st.buf('sg1')]
    psg = [st.ps([128, T], F32, f'pg{i}') for i in range(2)]; pgb = [st.buf(), st.buf()]
    psu = [st.ps([128, T], F32, f'pu{i}') for i in range(2)]; pub = [st.buf(), st.buf()]
    pso = [st.ps([128, T], F32, f'po{i}') for i in range(2)]; pob = [st.buf(), st.buf()]
    A, SH, HG = G['A'][:, ij, :], G['SH'][:, ij, :], G['HG'][:, ij, :]
    gb = G['gb']
    h, hb = R['h'], R['hb']
    if final:
        fng = st.sb([128, 8], F32, 'fng'); fngb = st.buf('fng')
        cx.dma('sp', fng[:, :], P['fng'], writes=[fngb])
    for i in range(L // T):
        s = i % 2
        xt = xs[s]
        cx.dma('sp', xt[:, :, :], x_in[:, :, i * T:(i + 1) * T], writes=[xb[s]])
        rms_mod(cx, st, R, xt[:, :, :], xb[s], A, SH, T)
        for fc in range(NFC):
            j = fc % 2
            for kc in range(8):
                cx.op('pe', lambda kc=kc, fc=fc, j=j: nc.tensor.matmul(
                    psg[j][:, :], wg[:, kc, fc * 128:(fc + 1) * 128], h[:, kc, :], start=(kc == 0), stop=(kc == 7)),
                    reads=[wgb[kc], hb], writes=[pgb[j]])
            for kc in range(8):
                cx.op('pe', lambda kc=kc, fc=fc, j=j: nc.tensor.matmul(
                    psu[j][:, :], wu[:, kc, fc * 128:(fc + 1) * 128], h[:, kc, :], start=(kc == 0), stop=(kc == 7)),
                    reads=[wub[kc], hb], writes=[pub[j]])
            cx.op('act', lambda j=j: nc.scalar.activation(out=sg[:, j, :], in_=psg[j][:, :], func=AF.Silu),
                  reads=[pgb[j]], writes=[sgb[j]])
            cx.op('dve', lambda j=j, fc=fc: nc.vector.tensor_tensor(out=act[:, fc, :], in0=psu[j][:, :], in1=sg[:, j, :],
                                                                    op=ALU.mult), reads=[pub[j], sgb[j]], writes=[actb[fc]])
        for dc in range(8):
            j = dc % 2
            for fc in range(NFC):
                cx.op('pe', lambda fc=fc, dc=dc, j=j: nc.tensor.matmul(
                    pso[j][:, :], wd[:, fc, dc * 128:(dc + 1) * 128], act[:, fc, :], start=(fc == 0), stop=(fc == NFC - 1)),
                    reads=[wdb[fc], actb[fc]], writes=[pob[j]])
            cx.op('dve', lambda dc=dc, j=j, xt=xt: nc.vector.scalar_tensor_tensor(
                out=xt[:, dc, :], in0=pso[j][:, :], scalar=HG[:, dc:dc + 1], in1=xt[:, dc, :], op0=ALU.mult, op1=ALU.add),
                reads=[pob[j], xb[s], gb], writes=[xb[s]])
        if final:
            final_norm(cx, st, R, xt, xb[s], fng, fngb, T)
        cx.dma('sp', x_out[:, :, i * T:(i + 1) * T], xt[:, :, :], reads=[xb[s]])
    st.close()


def final_norm(cx, st, R, xt, xb, fng, fngb, T):
    nc = cx.nc
    sq, sqb, pss, pssb, rstd, rstdb = (R[k] for k in ('sq', 'sqb', 'pss', 'pssb', 'rstd', 'rstdb'))
    ones = R['ones_bf']
    cx.op('act', lambda: nc.scalar.activation(out=sq[:, :, :], in_=xt[:, :, :], func=AF.Square), reads=[xb], writes=[sqb])
    for c in range(8):
        cx.op('pe', lambda c=c: nc.tensor.matmul(pss[:, :], ones[:, :], sq[:, c, :], start=(c == 0), stop=(c == 7)),
              reads=[sqb], writes=[pssb])
    cx.op('act', lambda: nc.scalar.activation(out=rstd[:, :], in_=pss[:, :], func=AF.Sqrt, bias=R['epsc'][:, 0:1],
                                              scale=1.0), reads=[pssb], writes=[rstdb])
    cx.op('dve', lambda: nc.vector.reciprocal(out=rstd[:, :], in_=rstd[:, :]), reads=[rstdb], writes=[rstdb])
    cx.op('dve', lambda: nc.vector.tensor_scalar(out=rstd[:, :], in0=rstd[:, :], scalar1=float(D) ** 0.5, scalar2=0.0,
                                                 op0=ALU.mult, op1=ALU.add), reads=[rstdb], writes=[rstdb])
    for c in range(8):
        cx.op('dve', lambda c=c: nc.vector.scalar_tensor_tensor(
            out=xt[:, c, :], in0=xt[:, c, :], scalar=fng[:, c:c + 1], in1=rstd[:, :], op0=ALU.mult, op1=ALU.mult),
            reads=[xb, rstdb, fngb], writes=[xb])


def build(L, plan, dbg=()):
    nc = bass.Bass("TRN2", target_bir_lowering=False)
    P = {}

    def din(name, shape, dt=F32):
        P[name] = nc.dram_tensor(name, list(shape), dt, kind="ExternalInput").ap()

    din('xT', [128, 8, L]); din('ccol', [128, 8]); din('modw', [6, D, 3 * D]); din('modb', [128, 6, 24])
    din('ng', [128, 6, 8]); din('fng', [128, 8])
    din('wg', [4, D, DFF]); din('wu', [4, D, DFF]); din('wd', [4, DFF, D])
    din('ssm_win', [D, SSM_IN]); din('convw', [128, 32, 4]); din('convb', [128, 32]); din('dtbias', [128, 32])
    din('alog', [128, 32]); din('dskip', [128, 32]); din('ssm_nw', [64, 32]); din('ssm_wout', [2048, D])
    din('ident', [128, 128]); din('identf', [128, 128]); din('triu', [128, 128])
    din('attn_wqkv', [D, 3 * D]); din('attn_wout', [D, D])
    din('EE', [64, 32, 128]); din('CM', [128, 4, 512]); din('abase', [128, 67]); din('qlc', [128, 4])
    S = {}
    zs_d = nc.dram_tensor('zs', [2048, L], F32).ap()
    S['zs'] = zs_d.rearrange("(c q) t -> q c t", q=128); S['zs64'] = zs_d.rearrange("(h p) t -> p h t", p=64)
    S['bct'] = nc.dram_tensor('bct', [2048, L], BF16).ap().rearrange("(c q) t -> q c t", q=128)
    S['xtok'] = nc.dram_tensor('xtok', [L, 3072], BF16).ap()
    S['dtt'] = nc.dram_tensor('dtt', [L, 32], F32).ap()
    if 'yT' in dbg:
        yT_d = nc.dram_tensor('yT', [2048, L], F32, kind="ExternalOutput").ap()
    else:
        yT_d = nc.dram_tensor('yT', [2048, L], F32).ap()
    S['yT'] = yT_d.rearrange("(h p) t -> p h t", p=64)
    kw_ = dict(kind="ExternalOutput") if 'dump' in dbg else {}
    if 'dump' in dbg:
        S['dbgsel'] = nc.dram_tensor('dbgsel', [L, 65], F32, kind="ExternalOutput").ap()
        S['dbgmb'] = nc.dram_tensor('dbgmb', [64, L], BF16, kind="ExternalOutput").ap()
    S['qT'] = nc.dram_tensor('qT', [D, L], F32, **kw_).ap()
    S['kT'] = nc.dram_tensor('kT', [D, L], BF16, **kw_).ap()
    S['vtok'] = nc.dram_tensor('vtok', [L, D], BF16, **kw_).ap()
    S['kmT'] = nc.dram_tensor('kmT', [D, 32], F32, **kw_).ap()
    if 'aT' in dbg:
        S['aT'] = nc.dram_tensor('aT', [D, L], F32, kind="ExternalOutput").ap()
    else:
        S['aT'] = nc.dram_tensor('aT', [D, L], F32).ap()
    P['out'] = nc.dram_tensor('outT', [128, 8, L], F32, kind="ExternalOutput").ap()
    xa = nc.dram_tensor('xa', [128, 8, L], F32).ap()
    xb_ = nc.dram_tensor('xb', [128, 8, L], F32).ap()
    cx = Ctx(nc)
    with ExitStack() as es:
        G = {}
        for k in ('A', 'SH', 'GT', 'HG'):
            G[k] = es.enter_context(nc.sbuf_tensor(f'g_{k}', [128, 6, 8], F32))
        G['gb'] = Buf('g')
        G['ones_bf'] = es.enter_context(nc.sbuf_tensor('g_ones', [128, 128], BF16))
        es.enter_context(nc.Block())
        ob = Buf('ones')
        cx.op('dve', lambda: nc.vector.memset(G['ones_bf'][:, :], 1.0), writes=[ob])
        G['epsc'] = es.enter_context(nc.sbuf_tensor('g_eps', [128, 2], F32))
        G['onec'] = es.enter_context(nc.sbuf_tensor('g_one', [128, 1], F32))
        cx.op('dve', lambda: nc.vector.memset(G['onec'][:, :], 1.0), writes=[ob])
        cx.op('dve', lambda: nc.vector.memset(G['epsc'][:, 0:1], float(D) * EPS), writes=[ob])
        cx.op('dve', lambda: nc.vector.memset(G['epsc'][:, 1:2], 2048.0 * EPS), writes=[ob])
        cx.barrier()
        cur = P['xT']
        pp = [xa, xb_]
        npp = 0
        for si, s in enumerate(plan):
            last = (si == len(plan) - 1)
            if s == 'mod':
                mod_stage(cx, P, G)
            elif s == 'mamba':
                dst = P['out'] if last else pp[npp % 2]
                m1_stage(cx, P, G, S, cur, L, T=128)
                m2_stage(cx, P, G, S, L)
                m3_stage(cx, P, G, S, cur, dst, L, T=128)
                cur = dst
                npp += 1
            elif s == 'moba':
                dst = P['out'] if last else pp[npp % 2]
                a1_stage(cx, P, G, S, cur, L, dbg=dbg)
                if 'a1only' not in dbg:
                    a2_stage(cx, P, G, S, L)
                if 'noa3' not in dbg:
                    a3_stage(cx, P, G, S, cur, dst, L)
                cur = dst
                npp += 1
            elif s.startswith('ffn'):
                fi = int(s[3])
                ij = (fi // 2) * 3 + (0 if fi % 2 == 0 else 2)
                dst = P['out'] if last else pp[npp % 2]
                ffn_stage(cx, P, G, cur, dst, fi, ij, L, final=(s.endswith('F')))
                cur = dst
                npp += 1
        cx.barrier()
    return nc


def colify(v, n):
    return np.ascontiguousarray(np.asarray(v, np.float32).reshape(n, 128).T)


def prep_core(b, inp, L):
    x = np.asarray(inp['x'][b, :L], np.float32)
    m = {}
    m['xT'] = np.ascontiguousarray(x.T.reshape(8, 128, L).transpose(1, 0, 2))
    m['ccol'] = colify(inp['c'][b], 8)
    m['modw'] = np.ascontiguousarray(np.asarray(inp['mod_w'], np.float32).reshape(6, D, 3 * D))
    mbv = np.asarray(inp['mod_b'], np.float32).reshape(6, 24, 128)
    m['modb'] = np.ascontiguousarray(mbv.transpose(2, 0, 1))
    ngv = np.asarray(inp['norm_g'], np.float32).reshape(6, 8, 128)
    m['ng'] = np.ascontiguousarray(ngv.transpose(2, 0, 1))
    m['fng'] = colify(inp['final_norm_g'], 8)
    m['wg'] = np.ascontiguousarray(np.asarray(inp['ffn_w_gate'], np.float32).reshape(4, D, DFF))
    m['wu'] = np.ascontiguousarray(np.asarray(inp['ffn_w_up'], np.float32).reshape(4, D, DFF))
    m['wd'] = np.ascontiguousarray(np.asarray(inp['ffn_w_down'], np.float32).reshape(4, DFF, D))
    m['ssm_win'] = np.ascontiguousarray(inp['ssm_w_in'][0])
    cwv = np.asarray(inp['ssm_conv_w'], np.float32)[0, :, 0, :]
    m['convw'] = np.ascontiguousarray(cwv.reshape(4, 32, 128).transpose(2, 1, 0))
    m['convb'] = colify(inp['ssm_conv_b'][0], 32)
    bc = lambda v: np.ascontiguousarray(np.broadcast_to(np.asarray(v, np.float32).reshape(1, 32), (128, 32)))
    m['dtbias'] = bc(inp['ssm_dt_bias'][0]); m['alog'] = bc(inp['ssm_a_log'][0]); m['dskip'] = bc(inp['ssm_d'][0])
    m['ssm_nw'] = np.ascontiguousarray(np.asarray(inp['ssm_norm_w'][0], np.float32).reshape(32, 64).T)
    m['ssm_wout'] = np.ascontiguousarray(inp['ssm_w_out'][0])
    m['ident'] = np.eye(128, dtype=np.float32); m['identf'] = np.eye(128, dtype=np.float32)
    m['triu'] = np.triu(np.ones((128, 128), np.float32))
    m['attn_wqkv'] = np.ascontiguousarray(inp['attn_w_qkv'][0]); m['attn_wout'] = np.ascontiguousarray(inp['attn_w_out'][0])
    m.update(attn_consts())
    return m


def attn_consts():
    EE = np.zeros((64, 32, 128), np.float32)
    for j in range(32):
        EE[j, j, :] = 1.0
        EE[32 + j, j, :] = 1.0
    CM = np.zeros((128, 4, 512), np.float32)
    p = np.arange(128)[:, None]
    ql = np.arange(256)[None, :]
    for kk in range(2):
        m_ = np.where(kk * 128 + p > ql, NEG, 0.0).astype(np.float32)
        CM[:, kk, 0:256] = m_
        CM[:, 2 + kk, 256:512] = m_
    abase = (np.arange(128, dtype=np.float32)[:, None] - 128.0 * (np.arange(67, dtype=np.float32)[None, :] - 3.0))
    qlc = (np.arange(4, dtype=np.float32)[None, :] * 128.0 + np.arange(128, dtype=np.float32)[:, None])
    return {'EE': EE, 'CM': CM, 'abase': np.ascontiguousarray(abase.astype(np.float32)),
            'qlc': np.ascontiguousarray(qlc.astype(np.float32))}


def unT(o, L):
    return np.ascontiguousarray(o.transpose(1, 0, 2).reshape(D, L).T)


NH = 32
SSM_IN = 6176


def m1_stage(cx, P, G, S, x_in, L, ij=1, T=256):
    nc = cx.nc
    st = Stage(cx, 'm1')
    win, winb = load_w_bf16(cx, st, P['ssm_win'], D, SSM_IN, name='win')
    R = norm_res(st, T); R['ones_bf'] = G['ones_bf']; R['epsc'] = G['epsc']
    h, hb = R['h'], R['hb']
    xs = [st.sb([128, 8, T], F32, f'x{i}') for i in range(2)]; xb = [st.buf(), st.buf()]
    cw = st.sb([128, 32, 4], F32, 'cw'); cb = st.sb([128, 32], F32, 'cb'); cwb = st.buf('cw')
    dtb = st.sb([128, 32], F32, 'dtb'); dtbb = st.buf('dtb')
    identb = st.sb([128, 128], BF16, 'identb'); idb = st.buf('id')
    cx.dma('sp', cw[:, :, :], P['convw'], writes=[cwb])
    cx.dma('sp', cb[:, :], P['convb'], writes=[cwb])
    cx.dma('sp', dtb[:, :], P['dtbias'], writes=[dtbb])
    load_cast_small(cx, st, identb, P['ident'], [128, 128], idb, 'idc')
    raw = st.sb([128, 32, T + 3], F32, 'raw'); rawb = [st.buf(f'raw{c}') for c in range(32)]
    for c in range(32):
        cx.op('dve', lambda c=c: nc.vector.memset(raw[:, c, 0:3], 0.0), writes=[rawb[c]])
    acc = st.sb([128, 4, T], F32, 'acc'); accb = [st.buf() for _ in range(4)]
    cv = st.sb([128, 32, T], BF16, 'cv'); cvb = [st.buf(f'cv{c}') for c in range(32)]
    zt = st.sb([128, 16, T], F32, 'zt'); ztb = st.buf('zt')
    tok = st.sb([128, T // 128, 3072], BF16, 'tok'); tokb = st.buf('tok')
    dtt = st.sb([128, T // 128, 32], F32, 'dtt'); dttb = st.buf('dtt')
    pm = [st.ps([128, T], F32, f'pm{i}') for i in range(2)]; pmb = [st.buf(), st.buf()]
    pt = [st.ps([128, 512], BF16, f'pt{i}') for i in range(2)]; ptb = [st.buf(), st.buf()]
    pd = st.ps([128, 32], F32, 'pd'); pdb = st.buf('pd')
    A, SH = G['A'][:, ij, :], G['SH'][:, ij, :]
    nmm = 0
    for i in range(L // T):
        s = i % 2
        xt = xs[s]
        cx.dma('sp', xt[:, :, :], x_in[:, :, i * T:(i + 1) * T], writes=[xb[s]])
        rms_mod(cx, st, R, xt[:, :, :], xb[s], A, SH, T)
        def mm_chunk(oc):
            nonlocal nmm
            j = nmm % 2
            nmm += 1
            for kc in range(8):
                cx.op('pe', lambda kc=kc: nc.tensor.matmul(
                    pm[j][:, :], win[:, kc, oc * 128:(oc + 1) * 128], h[:, kc, :], start=(kc == 0), stop=(kc == 7)),
                    reads=[winb[kc], hb], writes=[pmb[j]])
            return j

        for oc in range(16):
            j = mm_chunk(oc)
            cx.op('act', lambda oc=oc, j=j: nc.scalar.activation(out=zt[:, oc, :], in_=pm[j][:, :], func=AF.Silu),
                  reads=[pmb[j]], writes=[ztb])
        for c0 in range(0, 32, 4):
            pair = (c0, c0 + 1, c0 + 2, c0 + 3)
            for c in pair:
                j = mm_chunk(16 + c)
                cx.op('act', lambda c=c, j=j: nc.scalar.copy(out=raw[:, c, 3:T + 3], in_=pm[j][:, :]),
                      reads=[pmb[j]], writes=[rawb[c]])
            for c in pair:
                a = c % 4
                cx.op('dve', lambda c=c, a=a: nc.vector.tensor_scalar(
                    out=acc[:, a, :], in0=raw[:, c, 3:T + 3], scalar1=cw[:, c, 3:4], scalar2=cb[:, c:c + 1],
                    op0=ALU.mult, op1=ALU.add), reads=[rawb[c], cwb], writes=[accb[a]])
            for k in range(3):
                for c in pair:
                    a = c % 4
                    cx.op('dve', lambda c=c, a=a, k=k: nc.vector.scalar_tensor_tensor(
                        out=acc[:, a, :], in0=raw[:, c, k:k + T], scalar=cw[:, c, k:k + 1], in1=acc[:, a, :],
                        op0=ALU.mult, op1=ALU.add), reads=[rawb[c], cwb, accb[a]], writes=[accb[a]])
            for c in pair:
                a = c % 4
                cx.op('act', lambda c=c, a=a: nc.scalar.activation(out=cv[:, c, :], in_=acc[:, a, :], func=AF.Silu),
                      reads=[accb[a]], writes=[cvb[c]])
            for c in pair:
                cx.op('dve', lambda c=c: nc.vector.tensor_copy(out=raw[:, c, 0:3], in_=raw[:, c, T:T + 3]),
                      reads=[rawb[c]], writes=[rawb[c]])
        cx.dma('sp', S['zs'][:, :, i * T:(i + 1) * T], zt[:, :, :], reads=[ztb])
        cx.dma('sp', S['bct'][:, :, i * T:(i + 1) * T], cv[:, 16:32, :], reads=cvb[16:32])
        ntp = 0
        for sub in range(T // 128):
            for c4 in range(6):
                j = ntp % 2
                ntp += 1
                for q in range(4):
                    c = c4 * 4 + q
                    cx.op('pe', lambda c=c, q=q, j=j, sub=sub: nc.tensor.transpose(
                        pt[j][:, q * 128:(q + 1) * 128], cv[:, c, sub * 128:(sub + 1) * 128], identb[:, :]),
                        reads=[cvb[c], idb], writes=[ptb[j]])
                cx.op('act', lambda c4=c4, j=j, sub=sub: nc.scalar.copy(out=tok[:, sub, c4 * 512:(c4 + 1) * 512], in_=pt[j][:, :]),
                      reads=[ptb[j]], writes=[tokb])
            for kc in range(8):
                cx.op('pe', lambda kc=kc, sub=sub: nc.tensor.matmul(
                    pd[:, :], h[:, kc, sub * 128:(sub + 1) * 128], win[:, kc, 6144:6176], start=(kc == 0), stop=(kc == 7)),
                    reads=[winb[kc], hb], writes=[pdb])
            cx.op('dve', lambda sub=sub: nc.vector.tensor_tensor(out=dtt[:, sub, :], in0=pd[:, :], in1=dtb[:, :], op=ALU.add),
                  reads=[pdb, dtbb], writes=[dttb])
        cx.op('act', lambda: nc.scalar.activation(out=dtt[:, :, :], in_=dtt[:, :, :], func=AF.Exp), reads=[dttb], writes=[dttb])
        cx.op('act', lambda: nc.scalar.activation(out=dtt[:, :, :], in_=dtt[:, :, :], func=AF.Ln, bias=G['onec'][:, 0:1],
                                                  scale=1.0), reads=[dttb], writes=[dttb])
        cx.dma('sp', S['xtok'][i * T:(i + 1) * T, :].rearrange("(n p) c -> p n c", p=128), tok[:, :, :], reads=[tokb])
        cx.dma('sp', S['dtt'][i * T:(i + 1) * T, :].rearrange("(n p) c -> p n c", p=128), dtt[:, :, :], reads=[dttb])
    st.close()


def m2_stage(cx, P, G, S, L):
    nc = cx.nc
    st = Stage(cx, 'm2')
    Q = 128
    triu = st.sb([128, 128], F32, 'triu'); onesf = st.sb([128, 128], F32, 'onesf'); tri01 = st.sb([128, 128], F32, 'tri01')
    cb = st.buf('const')
    cx.dma('sp', triu[:, :], P['triu'], writes=[cb])
    cx.dma('sp', tri01[:, :], P['triu'], writes=[cb])
    cx.op('dve', lambda: nc.vector.memset(onesf[:, :], 1.0), writes=[cb])
    abc = st.sb([128, 32], F32, 'abc'); dsk = st.sb([128, 32], F32, 'dsk')
    cx.dma('sp', abc[:, :], P['alog'], writes=[cb])
    cx.dma('sp', dsk[:, :], P['dskip'], writes=[cb])
    identf = st.sb([128, 128], F32, 'identf')
    cx.dma('sp', identf[:, :], P['identf'], writes=[cb])
    cx.op('act', lambda: nc.scalar.activation(out=abc[:, :], in_=abc[:, :], func=AF.Exp), reads=[cb], writes=[cb])
    cx.op('dve', lambda: nc.vector.tensor_scalar(out=abc[:, :], in0=abc[:, :], scalar1=-1.0, scalar2=0.0, op0=ALU.mult,
                                                 op1=ALU.add), reads=[cb], writes=[cb])
    DI = st.sb([128, 32, 128], BF16, 'DI')
    cx.op('dve', lambda: nc.vector.tensor_tensor(out=DI[:, :, :], in0=identf[:, :].unsqueeze(1).to_broadcast([128, 32, 128]),
                                                 in1=dsk[:, :].unsqueeze(2).to_broadcast([128, 32, 128]), op=ALU.mult),
          reads=[cb], writes=[cb])
    St = st.sb([128, 32, 64], F32, 'St'); Sbf = st.sb([128, 32, 64], BF16, 'Sbf'); Sb = st.buf('S'); Sbb = st.buf('Sbf')
    cx.op('dve', lambda: nc.vector.memset(St[:, :, :], 0.0), writes=[Sb])
    cx.op('dve', lambda: nc.vector.memset(Sbf[:, :, :], 0.0), writes=[Sbb])
    xt = [st.sb([128, 3072], BF16, f'xt{i}') for i in range(2)]; xtb = [st.buf(), st.buf()]
    bc = [st.sb([128, 16, Q], BF16, f'bc{i}') for i in range(2)]; bcb = [st.buf(), st.buf()]
    dt = [st.sb([128, 32], F32, f'dt{i}') for i in range(2)]; dtb = [st.buf(), st.buf()]
    sm = st.sb([128, 8, 32], F32, 'sm'); smb = st.buf('sm')
    xw = st.sb([128, 32, 64], BF16, 'xw'); xwb = st.buf('xw')
    rhsA = st.sb([128, 4, 128], F32, 'rhsA'); rhsAb = st.buf('rhsA')
    E1 = st.sb([128, 4, 128], F32, 'E1'); E1b = st.buf('E1')
    cdec = st.sb([128, 4, 128], BF16, 'cdec'); cdecb = st.buf('cdec')
    pre = st.sb([128, 4, 128], F32, 'pre'); preb = st.buf('pre')
    cbm = st.sb([128, 128], F32, 'cbm'); cbmb = st.buf('cbm')
    WT = st.sb([128, 4, 128], BF16, 'WT'); WTb = st.buf('WT')
    ysb = [st.sb([64, 32, Q], F32, f'ysb{i}') for i in range(2)]; ysbb = [st.buf(), st.buf()]
    p_sm = st.ps([128, 2, 32], F32, 'psm'); p_smb = st.buf()
    p_cb = st.ps([128, 128], F32, 'pcb'); p_cbb = st.buf()
    p_ac = st.ps([128, 512], F32, 'pac'); p_acb = st.buf()
    p_y = [st.ps([64, Q], F32, f'py{i}') for i in range(2)]; p_yb = [st.buf(), st.buf()]
    p_s = [st.ps([128, 256], F32, f'pS{i}') for i in range(2)]; p_sb = [st.buf(), st.buf()]
    ny = 0
    for ci in range(L // Q):
        s = ci % 2
        t0 = ci * Q
        cx.dma('sp', xt[s][:, :], S['xtok'][t0:t0 + Q, :], writes=[xtb[s]])
        cx.dma('sp', bc[s][:, :, :], S['bct'][:, :, t0:t0 + Q], writes=[bcb[s]])
        cx.dma('sp', dt[s][:, :], S['dtt'][t0:t0 + Q, :], writes=[dtb[s]])
        dtA, lndt, bcol, wend, cdc = sm[:, 0, :], sm[:, 1, :], sm[:, 2, :], sm[:, 3, :], sm[:, 4, :]
        cx.op('dve', lambda: nc.vector.tensor_tensor(out=dtA, in0=dt[s][:, :], in1=abc[:, :], op=ALU.mult),
              reads=[dtb[s], cb], writes=[smb])
        cx.op('pe', lambda: nc.tensor.matmul(p_sm[:, 0, :], triu[:, :], dtA, start=True, stop=True), reads=[smb, cb], writes=[p_smb])
        cx.op('pe', lambda: nc.tensor.matmul(p_sm[:, 1, :], onesf[:, :], dtA, start=True, stop=True), reads=[smb, cb], writes=[p_smb])
        cx.op('act', lambda: nc.scalar.activation(out=lndt, in_=dt[s][:, :], func=AF.Ln), reads=[dtb[s]], writes=[smb])
        cx.op('dve', lambda: nc.vector.tensor_tensor(out=bcol, in0=lndt, in1=p_sm[:, 0, :], op=ALU.subtract),
              reads=[smb, p_smb], writes=[smb])
        cx.op('dve', lambda: nc.vector.tensor_tensor(out=wend, in0=bcol, in1=p_sm[:, 1, :], op=ALU.add),
              reads=[smb, p_smb], writes=[smb])
        cx.op('act', lambda: nc.scalar.activation(out=wend, in_=wend, func=AF.Exp), reads=[smb], writes=[smb])
        cx.op('act', lambda: nc.scalar.activation(out=cdc, in_=p_sm[:, 1, :], func=AF.Exp), reads=[p_smb], writes=[smb])
        cx.op('dve', lambda: nc.vector.tensor_tensor(
            out=xw[:, :, :], in0=xt[s][:, 0:2048].rearrange("p (h q) -> p h q", h=32),
            in1=wend.unsqueeze(2).to_broadcast([128, 32, 64]), op=ALU.mult), reads=[xtb[s], smb], writes=[xwb])
        ys = ysb[s]
        for g in range(8):
            BT = bc[s][:, g, :]
            CT = bc[s][:, 8 + g, :]
            cx.op('pe', lambda: nc.tensor.matmul(p_cb[:, :], BT, CT, start=True, stop=True), reads=[bcb[s]], writes=[p_cbb])
            cx.op('dve', lambda: nc.vector.tensor_tensor(out=cbm[:, :], in0=p_cb[:, :], in1=tri01[:, :], op=ALU.mult),
                  reads=[p_cbb, cb], writes=[cbmb])
            cx.op('dve', lambda g=g: nc.vector.tensor_tensor(
                out=rhsA[:, :, :], in0=triu[:, :].unsqueeze(1).to_broadcast([128, 4, 128]),
                in1=dtA[:, 4 * g:4 * g + 4].unsqueeze(2).to_broadcast([128, 4, 128]), op=ALU.mult),
                reads=[smb, cb], writes=[rhsAb])
            cx.op('pe', lambda: nc.tensor.matmul(p_ac[:, :], onesf[:, :], rhsA[:, :, :].rearrange("p h t -> p (h t)"),
                                                 start=True, stop=True), reads=[rhsAb, cb], writes=[p_acb])
            cx.op('act', lambda: nc.scalar.activation(out=E1[:, :, :].rearrange("p h t -> p (h t)"), in_=p_ac[:, :], func=AF.Exp),
                  reads=[p_acb], writes=[E1b])
            cx.op('dve', lambda: nc.vector.tensor_tensor(out=cdec[:, :, :], in0=E1[:, :, :],
                                                         in1=CT.unsqueeze(1).to_broadcast([128, 4, 128]), op=ALU.mult),
                  reads=[E1b, bcb[s]], writes=[cdecb])
            for hh in range(4):
                hd = 4 * g + hh
                cx.op('dve', lambda hh=hh, hd=hd: nc.vector.tensor_scalar(
                    out=pre[:, hh, :], in0=p_ac[:, hh * 128:(hh + 1) * 128], scalar1=bcol[:, hd:hd + 1], scalar2=20.0,
                    op0=ALU.add, op1=ALU.min), reads=[p_acb, smb], writes=[preb])
            cx.op('act', lambda: nc.scalar.activation(out=pre[:, :, :], in_=pre[:, :, :], func=AF.Exp), reads=[preb], writes=[preb])
            cx.op('dve', lambda: nc.vector.tensor_tensor(out=WT[:, :, :], in0=pre[:, :, :],
                                                         in1=cbm[:, :].unsqueeze(1).to_broadcast([128, 4, 128]), op=ALU.mult),
                  reads=[preb, cbmb], writes=[WTb])
            for hh in range(4):
                hd = 4 * g + hh
                j = ny % 2
                ny += 1
                xh = xt[s][:, hd * 64:(hd + 1) * 64]
                cx.op('pe', lambda hh=hh, j=j, xh=xh: nc.tensor.matmul(p_y[j][:, :], xh, WT[:, hh, :], start=True, stop=False),
                      reads=[xtb[s], WTb], writes=[p_yb[j]])
                cx.op('pe', lambda hd=hd, j=j, xh=xh: nc.tensor.matmul(p_y[j][:, :], xh, DI[:, hd, :], start=False, stop=False),
                      reads=[xtb[s], cb], writes=[p_yb[j]])
                cx.op('pe', lambda hh=hh, hd=hd, j=j: nc.tensor.matmul(p_y[j][:, :], Sbf[:, hd, :], cdec[:, hh, :], start=False, stop=True),
                      reads=[Sbb, cdecb], writes=[p_yb[j]])
                cx.op('act', lambda hd=hd, j=j, ys=ys: nc.scalar.copy(out=ys[:, hd, :], in_=p_y[j][:, :]),
                      reads=[p_yb[j]], writes=[ysbb[s]])
        cx.dma('sp', S['yT'][:, :, t0:t0 + Q], ys[:, :, :], reads=[ysbb[s]])
        cx.op('dve', lambda: nc.vector.tensor_tensor(out=St[:, :, :], in0=St[:, :, :],
                                                     in1=cdc.unsqueeze(2).to_broadcast([128, 32, 64]), op=ALU.mult),
              reads=[Sb, smb], writes=[Sb])
        for g in range(8):
            j = g % 2
            cx.op('pe', lambda g=g, j=j: nc.tensor.matmul(
                p_s[j][:, :], xt[s][:, 2048 + g * 128:2048 + (g + 1) * 128],
                xw[:, 4 * g:4 * g + 4, :].rearrange("p h q -> p (h q)"), start=True, stop=True),
                reads=[xtb[s], xwb], writes=[p_sb[j]])
            cx.op('dve', lambda g=g, j=j: nc.vector.tensor_tensor(
                out=St[:, 4 * g:4 * g + 4, :].rearrange("p h q -> p (h q)"),
                in0=St[:, 4 * g:4 * g + 4, :].rearrange("p h q -> p (h q)"), in1=p_s[j][:, :], op=ALU.add),
                reads=[Sb, p_sb[j]], writes=[Sb])
        cx.op('act', lambda: nc.scalar.copy(out=Sbf[:, :, :], in_=St[:, :, :]), reads=[Sb], writes=[Sbb])
    st.close()


def m3_stage(cx, P, G, S, x_in, x_out, L, ij=1, T=256):
    nc = cx.nc
    st = Stage(cx, 'm3')
    wo = st.sb([64, 32, D], BF16, 'wo'); wob = [st.buf(f'wo{h}') for h in range(32)]
    wstg = st.sb([64, 2, 4, D], F32, 'wstg'); wstgb = [st.buf(), st.buf()]
    for hgrp in range(8):
        j = hgrp % 2
        cx.dma('sp', wstg[:, j, :, :], P['ssm_wout'][hgrp * 256:(hgrp + 1) * 256, :].rearrange("(h p) d -> p h d", p=64),
               writes=[wstgb[j]])
        cx.op('act', lambda hgrp=hgrp, j=j: nc.scalar.copy(out=wo[:, hgrp * 4:(hgrp + 1) * 4, :], in_=wstg[:, j, :, :]),
              reads=[wstgb[j]], writes=wob[hgrp * 4:(hgrp + 1) * 4])
    nw = st.sb([64, 32], F32, 'nw'); nwb = st.buf('nw')
    cx.dma('sp', nw[:, :], P['ssm_nw'], writes=[nwb])
    ones64 = G['ones_bf']
    ys = [st.sb([64, 32, T], F32, f'y{i}') for i in range(2)]; ysb = [st.buf(), st.buf()]
    zs = [st.sb([64, 32, T], F32, f'z{i}') for i in range(2)]; zsb = [st.buf(), st.buf()]
    xs = [st.sb([128, 8, T], F32, f'x{i}') for i in range(2)]; xb = [st.buf(), st.buf()]
    sq = st.sb([64, 32, T], BF16, 'sq'); sqb = st.buf('sq')
    yn = st.sb([64, 32, T], BF16, 'yn'); ynb = st.buf('yn')
    rstd = st.sb([64, T], F32, 'rstd'); rstdb = st.buf('rstd')
    pss = st.ps([64, T], F32, 'pss'); pssb = st.buf()
    po = [st.ps([128, T], F32, f'po{i}') for i in range(2)]; pob = [st.buf(), st.buf()]
    GT = G['GT'][:, ij, :]
    for i in range(L // T):
        s = i % 2
        sl = slice(i * T, (i + 1) * T)
        cx.dma('sp', ys[s][:, :, :], S['yT'][:, :, sl], writes=[ysb[s]])
        cx.dma('sp', zs[s][:, :, :], S['zs'][:, :, sl].rearrange("q c t -> q c t") if False else
               S['zs64'][:, :, sl], writes=[zsb[s]])
        cx.dma('sp', xs[s][:, :, :], x_in[:, :, sl], writes=[xb[s]])
        cx.op('dve', lambda: nc.vector.tensor_tensor(out=ys[s][:, :, :], in0=ys[s][:, :, :], in1=zs[s][:, :, :], op=ALU.mult),
              reads=[ysb[s], zsb[s]], writes=[ysb[s]])
        cx.op('act', lambda: nc.scalar.activation(out=sq[:, :, :], in_=ys[s][:, :, :], func=AF.Square), reads=[ysb[s]], writes=[sqb])
        for hd in range(32):
            cx.op('pe', lambda hd=hd: nc.tensor.matmul(pss[:, :], ones64[0:64, 0:64], sq[:, hd, :], start=(hd == 0), stop=(hd == 31)),
                  reads=[sqb], writes=[pssb])
        cx.op('act', lambda: nc.scalar.activation(out=rstd[:, :], in_=pss[:, :], func=AF.Sqrt, bias=G['epsc'][0:64, 1:2],
                                                  scale=1.0), reads=[pssb], writes=[rstdb])
        cx.op('dve', lambda: nc.vector.reciprocal(out=rstd[:, :], in_=rstd[:, :]), reads=[rstdb], writes=[rstdb])
        for hd in range(32):
            cx.op('dve', lambda hd=hd: nc.vector.scalar_tensor_tensor(
                out=ys[s][:, hd, :], in0=ys[s][:, hd, :], scalar=nw[:, hd:hd + 1], in1=rstd[:, :], op0=ALU.mult, op1=ALU.mult),
                reads=[ysb[s], nwb, rstdb], writes=[ysb[s]])
        cx.op('act', lambda: nc.scalar.activation(out=yn[:, :, :], in_=ys[s][:, :, :], func=AF.Copy, scale=float(2048.0 ** 0.5)),
              reads=[ysb[s]], writes=[ynb])
        for dc in range(8):
            j = dc % 2
            for hd in range(32):
                cx.op('pe', lambda hd=hd, dc=dc, j=j: nc.tensor.matmul(
                    po[j][:, :], wo[:, hd, dc * 128:(dc + 1) * 128], yn[:, hd, :], start=(hd == 0), stop=(hd == 31)),
                    reads=[wob[hd], ynb], writes=[pob[j]])
            cx.op('dve', lambda dc=dc, j=j: nc.vector.scalar_tensor_tensor(
                out=xs[s][:, dc, :], in0=po[j][:, :], scalar=GT[:, dc:dc + 1], in1=xs[s][:, dc, :], op0=ALU.mult, op1=ALU.add),
                reads=[pob[j], xb[s], G['gb']], writes=[xb[s]])
        cx.dma('sp', x_out[:, :, sl], xs[s][:, :, :], reads=[xb[s]])
    st.close()


SEQ = 8192
PLAN = ['mod', 'ffn0', 'mamba', 'ffn1', 'ffn2', 'moba', 'ffn3F']


def kernel(**inputs):
    L = SEQ
    nc = build(L, PLAN)
    maps = [prep_core(b, inputs, L) for b in range(4)]
    in_maps = [maps[c % 4] for c in range(8)]
    res = run_bass_kernel_spmd(nc, in_maps, core_ids=list(range(8)))
    out = np.stack([unT(res.results[b]['outT'], L) for b in range(4)], axis=0)
    return out.astype(np.float32)


NHA = 16
SLOPES = [2.0 ** (-8.0 * (hh + 1) / 16.0) for hh in range(16)]
NEG = -30000.0


def a1_stage(cx, P, G, S, x_in, L, ij=4, T=256, dbg=()):
    nc = cx.nc
    st = Stage(cx, 'a1')
    wq, wqb = load_w_bf16(cx, st, P['attn_wqkv'], D, 3 * D, name='wqkv')
    R = norm_res(st, T); R['ones_bf'] = G['ones_bf']; R['epsc'] = G['epsc']
    h, hb = R['h'], R['hb']
    xs = [st.sb([128, 8, T], F32, f'x{i}') for i in range(2)]; xb = [st.buf(), st.buf()]
    qt = st.sb([128, 8, T], F32, 'qt'); qtb = st.buf('qt')
    kt = st.sb([128, 8, T], BF16, 'kt'); ktb = st.buf('kt')
    vt = st.sb([128, T // 128, 1024], BF16, 'vt'); vtb = st.buf('vt')
    NB = L // 256
    kms = st.sb([128, 8, 32], F32, 'kms'); kmsb = st.buf('kms')
    kf = st.sb([128, 8, 128], F32, 'kf'); kfb = st.buf('kf')
    cx.op('dve', lambda: nc.vector.memset(kms[:, :, :], 0.0), writes=[kmsb])
    pm = [st.ps([128, T], F32, f'pm{i}') for i in range(2)]; pmb = [st.buf(), st.buf()]
    pv = [st.ps([128, 512], F32, f'pv{i}') for i in range(2)]; pvb = [st.buf(), st.buf()]
    A, SH = G['A'][:, ij, :], G['SH'][:, ij, :]
    qT_v = S['qT'].rearrange("(c q) t -> q c t", q=128)
    kT_v = S['kT'].rearrange("(c q) t -> q c t", q=128)
    n = 0
    nv = 0
    for i in range(L // T):
        s = i % 2
        xt = xs[s]
        sl = slice(i * T, (i + 1) * T)
        cx.dma('sp', xt[:, :, :], x_in[:, :, sl], writes=[xb[s]])
        rms_mod(cx, st, R, xt[:, :, :], xb[s], A, SH, T)
        for oc in range(16):
            j = n % 2
            n += 1
            for kc in range(8):
                cx.op('pe', lambda kc=kc, oc=oc, j=j: nc.tensor.matmul(
                    pm[j][:, :], wq[:, kc, oc * 128:(oc + 1) * 128], h[:, kc, :], start=(kc == 0), stop=(kc == 7)),
                    reads=[wqb[kc], hb], writes=[pmb[j]])
            if oc < 8:
                cx.op('act', lambda oc=oc, j=j: nc.scalar.activation(out=qt[:, oc, :], in_=pm[j][:, :], func=AF.Copy, scale=0.125),
                      reads=[pmb[j]], writes=[qtb])
            else:
                c = oc - 8
                cx.op('act', lambda c=c, j=j: nc.scalar.copy(out=kt[:, c, :], in_=pm[j][:, :]), reads=[pmb[j]], writes=[ktb])
        if 'nokms' not in dbg:
            cx.op('dve', lambda: nc.vector.tensor_tensor(out=kf[:, :, :], in0=kt[:, :, 0:128], in1=kt[:, :, 128:256], op=ALU.add),
                  reads=[ktb], writes=[kfb])
            w = 64
            while w >= 1:
                cx.op('dve', lambda w=w: nc.vector.tensor_tensor(out=kf[:, :, 0:w], in0=kf[:, :, 0:w], in1=kf[:, :, w:2 * w], op=ALU.add),
                      reads=[kfb], writes=[kfb])
                w //= 2
            cx.op('dve', lambda i=i: nc.vector.tensor_copy(out=kms[:, :, i:i + 1], in_=kf[:, :, 0:1]), reads=[kfb], writes=[kmsb])
        for sub in range(T // 128 if 'nov' not in dbg else 0):
            for half in range(2):
                j = nv % 2
                nv += 1
                for kc in range(8):
                    cx.op('pe', lambda kc=kc, sub=sub, half=half, j=j: nc.tensor.matmul(
                        pv[j][:, :], h[:, kc, sub * 128:(sub + 1) * 128], wq[:, kc, 2048 + half * 512:2048 + (half + 1) * 512],
                        start=(kc == 0), stop=(kc == 7)), reads=[wqb[kc], hb], writes=[pvb[j]])
                cx.op('act', lambda sub=sub, half=half, j=j: nc.scalar.copy(out=vt[:, sub, half * 512:(half + 1) * 512], in_=pv[j][:, :]),
                      reads=[pvb[j]], writes=[vtb])
        cx.dma('sp', qT_v[:, :, sl], qt[:, :, :], reads=[qtb])
        cx.dma('sp', kT_v[:, :, sl], kt[:, :, :], reads=[ktb])
        if 'nov' not in dbg:
            cx.dma('sp', S['vtok'][sl, :].rearrange("(n p) c -> p n c", p=128), vt[:, :, :], reads=[vtb])
    if 'nokms' not in dbg:
        for c in range(8):
            cx.dma('sp', S['kmT'][c * 128:(c + 1) * 128, :], kms[:, c, :], reads=[kmsb])
    st.close()


def a2_stage(cx, P, G, S, L):
    nc = cx.nc
    st = Stage(cx, 'a2')
    NT, NQ, NB = L // 128, L // 512, L // 256
    ND = NT + 3
    cb = st.buf('const')
    EE = st.sb([64, 32, 128], BF16, 'EE'); CM = st.sb([128, 4, 512], BF16, 'CM'); identb = st.sb([128, 128], BF16, 'idb')
    load_cast_small(cx, st, EE, P['EE'], [64, 32, 128], cb, 'EEc')
    load_cast_small(cx, st, CM, P['CM'], [128, 4, 512], cb, 'CMc')
    load_cast_small(cx, st, identb, P['ident'], [128, 128], cb, 'idc')
    base = st.sb([128, ND], F32, 'base'); qlc = st.sb([128, 4], F32, 'qlc'); identf = st.sb([128, 128], F32, 'idf')
    cx.dma('sp', base[:, :], P['abase'][:, 0:ND], writes=[cb])
    cx.dma('sp', qlc[:, :], P['qlc'], writes=[cb])
    cx.dma('sp', identf[:, :], P['identf'], writes=[cb])
    thr0 = st.sb([128, 1], F32, 'thr0')
    cx.op('dve', lambda: nc.vector.memset(thr0[:, :], -1e29), writes=[cb])
    ones64 = G['ones_bf'][:, 0:64]
    kth = [st.sb([64, L], BF16, f'kth{i}') for i in range(2)]; kthb = [st.buf(), st.buf()]
    vh = [st.sb([128, NT, 64], BF16, f'vh{i}') for i in range(2)]
    vhb = [[st.buf() for _ in range(NT // 4)] for _ in range(2)]
    km = [st.sb([64, 32], F32, f'km{i}') for i in range(2)]; kmb = [st.buf(), st.buf()]
    alib = st.sb([128, ND], F32, 'alib'); alibb = st.buf('alib')
    kam = st.sb([64, 4], F32, 'kam'); kamb = st.buf('kam')
    kmx = st.sb([64, L // 2], BF16, 'kmx'); kmn = st.sb([64, L // 2], BF16, 'kmn')
    kamh = st.sb([64, 1], BF16, 'kamh'); kamhb = st.buf('kamh')
    qf = [st.sb([64, 512], F32, f'qf{i}') for i in range(2)]; qfb = [st.buf(), st.buf()]
    qb = st.sb([64, 512], BF16, 'qb'); qbb = st.buf('qb')
    qa = st.sb([64, 512], BF16, 'qa'); qab = st.buf('qa')
    gm = st.sb([128, 32], F32, 'gm'); m8 = st.sb([128, 8], F32, 'm8'); sel = st.sb([128, 32], F32, 'sel')
    sel2 = st.sb([128, 64], BF16, 'sel2'); r1 = st.sb([128, 32], F32, 'r1')
    rowc = st.sb([128, 1], F32, 'rowc')
    tb = st.buf('tk')
    sel2b = st.buf('sel2')
    MB2 = st.sb([64, 512], BF16, 'MB2'); MB2b = st.buf('MB2')
    pt = [st.sb([128, 512], BF16, f'pt{i}') for i in range(3)]; ptb = [st.buf() for _ in range(3)]
    osb = st.sb([64, 512], F32, 'osb'); osbb = st.buf('osb')
    rden = st.sb([64, 512], F32, 'rden'); rdenb = st.buf('rden')
    psc = [st.ps([128, 512], F32, f'psc{i}') for i in range(2)]; pscb = [st.buf(), st.buf()]
    pnum = st.ps([64, 512], F32, 'pnum'); pnumb = st.buf()
    pden = st.ps([64, 512], F32, 'pden'); pdenb = st.buf()
    pg = st.ps([128, 64], F32, 'pg'); pgb = st.buf()
    ptr = st.ps([64, 512], BF16, 'ptr'); ptrb = st.buf()
    nsc = 0
    npt = 0
    nq = 0
    for hd in range(NHA):
        s = hd % 2
        slope = SLOPES[hd]
        cx.dma('sp', kth[s][:, :], S['kT'][hd * 64:(hd + 1) * 64, :], writes=[kthb[s]])
        vsrc = S['vtok'][:, hd * 64:(hd + 1) * 64].rearrange("(t p) c -> p t c", p=128)
        for vc in range(NT // 4):
            cx.dma('sp', vh[s][:, vc * 4:(vc + 1) * 4, :], vsrc[:, vc * 4:(vc + 1) * 4, :], writes=[vhb[s][vc]])
        cx.dma('sp', km[s][:, :], S['kmT'][hd * 64:(hd + 1) * 64, :], writes=[kmb[s]])
        for fo, fop in ((kmx, ALU.max), (kmn, ALU.min)):
            w = L // 2
            cx.op('dve', lambda fo=fo, fop=fop, w=w: nc.vector.tensor_tensor(out=fo[:, 0:w], in0=kth[s][:, 0:w], in1=kth[s][:, w:2 * w], op=fop),
                  reads=[kthb[s]], writes=[kamb])
            w //= 2
            while w >= 1:
                cx.op('dve', lambda fo=fo, fop=fop, w=w: nc.vector.tensor_tensor(out=fo[:, 0:w], in0=fo[:, 0:w], in1=fo[:, w:2 * w], op=fop),
                      reads=[kamb], writes=[kamb])
                w //= 2
        cx.op('dve', lambda: nc.vector.scalar_tensor_tensor(out=kam[:, 2:3], in0=kmn[:, 0:1], scalar=-1.0, in1=kmx[:, 0:1],
                                                            op0=ALU.mult, op1=ALU.max), reads=[kamb], writes=[kamb])
        cx.op('dve', lambda: nc.vector.tensor_copy(out=kamh[:, :], in_=kam[:, 2:3]), reads=[kamb], writes=[kamhb])
        cx.op('dve', lambda: nc.vector.tensor_scalar(out=alib[:, :], in0=base[:, :], scalar1=float(slope), scalar2=0.0,
                                                     op0=ALU.mult, op1=ALU.add), reads=[cb], writes=[alibb])
        for qi in range(NQ):
            q0 = qi * 512
            s2 = nq % 2
            nq += 1
            cx.dma('sp', qf[s2][:, :], S['qT'][hd * 64:(hd + 1) * 64, q0:q0 + 512], writes=[qfb[s2]])
            cx.op('act', lambda: nc.scalar.copy(out=qb[:, :], in_=qf[s2][:, :]), reads=[qfb[s2]], writes=[qbb])
            cx.op('dve', lambda: nc.vector.scalar_tensor_tensor(out=qa[:, :], in0=qf[s2][:, :], scalar=-1.0, in1=qf[s2][:, :],
                                                                op0=ALU.mult, op1=ALU.max), reads=[qfb[s2]], writes=[qab])
            for sub in range(4):
                b = (q0 + sub * 128) // 256
                qs = slice(sub * 128, (sub + 1) * 128)
                cx.op('pe', lambda qs=qs: nc.tensor.matmul(pg[:, 0:32], qf[s2][:, qs], km[s][:, :], start=True, stop=True),
                      reads=[qfb[s2], kmb[s]], writes=[pgb])
                cx.op('pe', lambda qs=qs: nc.tensor.matmul(pg[:, 32:33], qa[:, qs], kamh[:, :], start=True, stop=True),
                      reads=[qab, kamhb], writes=[pgb])
                cx.op('dve', lambda sub=sub: nc.vector.scalar_tensor_tensor(
                    out=rowc[:, :], in0=qlc[:, sub:sub + 1], scalar=-float(slope), in1=pg[:, 32:33], op0=ALU.mult, op1=ALU.subtract),
                    reads=[cb, pgb], writes=[tb])
                cx.op('dve', lambda: nc.vector.memset(gm[:, :], -1e30), writes=[tb])
                if b > 0:
                    cx.op('dve', lambda b=b: nc.vector.tensor_copy(out=gm[:, 0:b], in_=pg[:, 0:b]), reads=[pgb], writes=[tb])
                if b >= 3:
                    cx.op('dve', lambda: nc.vector.max(out=m8[:, :], in_=gm[:, :]), reads=[tb], writes=[tb])
                    thr = m8[:, 2:3]
                else:
                    thr = thr0[:, 0:1]
                cx.op('dve', lambda thr=thr: nc.vector.tensor_scalar(out=sel[:, :], in0=gm[:, :], scalar1=thr, scalar2=-NEG,
                                                                     op0=ALU.is_ge, op1=ALU.mult), reads=[tb, cb], writes=[tb])
                cx.op('dve', lambda: nc.vector.tensor_scalar(out=sel[:, :], in0=sel[:, :], scalar1=NEG, scalar2=rowc[:, 0:1],
                                                             op0=ALU.add, op1=ALU.add), reads=[tb], writes=[tb])
                cx.op('dve', lambda b=b: nc.vector.tensor_copy(out=sel[:, b:b + 1], in_=rowc[:, :]), reads=[tb], writes=[tb])
                if 'dbgsel' in S and hd == 5:
                    r0 = q0 + sub * 128
                    cx.dma('sp', S['dbgsel'][r0:r0 + 128, 0:32], sel[:, :], reads=[tb])
                    cx.dma('sp', S['dbgsel'][r0:r0 + 128, 32:64], gm[:, :], reads=[tb])
                    cx.dma('sp', S['dbgsel'][r0:r0 + 128, 64:65], rowc[:, :], reads=[tb], allow_slow_non_contiguous=True)
                cx.op('dve', lambda: nc.vector.tensor_copy(out=sel2[:, 0:32], in_=sel[:, :]), reads=[tb, sel2b], writes=[tb, sel2b])
                cx.op('dve', lambda: nc.vector.tensor_tensor(out=r1[:, :], in0=sel[:, :], in1=sel2[:, 0:32], op=ALU.subtract),
                      reads=[tb, sel2b], writes=[tb])
                cx.op('dve', lambda: nc.vector.tensor_copy(out=sel2[:, 32:64], in_=r1[:, :]), reads=[tb, sel2b], writes=[tb, sel2b])
                cx.op('pe', lambda qs=qs: nc.tensor.transpose(ptr[:, qs], sel2[:, :], identb[:, :]), reads=[sel2b, cb], writes=[ptrb])
            cx.op('act', lambda: nc.scalar.copy(out=MB2[:, :], in_=ptr[:, :]), reads=[ptrb], writes=[MB2b])
            if 'dbgsel' in S and hd == 5:
                cx.dma('sp', S['dbgmb'][:, q0:q0 + 512], MB2[:, :], reads=[MB2b])
            nkt = 4 * qi + 4

            def emit_pv(ki, t, nkt=nkt):
                cx.op('pe', lambda: nc.tensor.matmul(pnum[:, :], vh[s][:, ki, :], pt[t][:, :], start=(ki == 0), stop=(ki == nkt - 1)),
                      reads=[vhb[s][ki // 4], ptb[t]], writes=[pnumb])
                cx.op('pe', lambda: nc.tensor.matmul(pden[:, :], ones64, pt[t][:, :], start=(ki == 0), stop=(ki == nkt - 1)),
                      reads=[ptb[t]], writes=[pdenb])

            prev = None
            for ki in range(nkt):
                j = ki // 2
                kk = ki % 2
                ddi = (q0 - ki * 128) // 128 + 3
                a = nsc % 2
                nsc += 1
                diag = j >= 2 * qi
                cx.op('pe', lambda ki=ki, a=a: nc.tensor.matmul(psc[a][:, :], kth[s][:, ki * 128:(ki + 1) * 128], qb[:, :],
                                                                 start=True, stop=False), reads=[kthb[s], qbb], writes=[pscb[a]])
                cx.op('pe', lambda j=j, a=a, diag=diag: nc.tensor.matmul(psc[a][:, :], EE[:, j, :], MB2[:, :], start=False, stop=(not diag)),
                      reads=[cb, MB2b], writes=[pscb[a]])
                if diag:
                    ci = (j - 2 * qi) * 2 + kk
                    cx.op('pe', lambda ci=ci, a=a: nc.tensor.matmul(psc[a][:, :], identb[:, :], CM[:, ci, :], start=False, stop=True),
                          reads=[cb], writes=[pscb[a]])
                if prev is not None:
                    emit_pv(*prev)
                t = npt % 3
                npt += 1
                cx.op('act', lambda a=a, t=t, ddi=ddi: nc.scalar.activation(out=pt[t][:, :], in_=psc[a][:, :], func=AF.Exp,
                                                                           bias=alib[:, ddi:ddi + 1], scale=1.0),
                      reads=[pscb[a], alibb], writes=[ptb[t]])
                prev = (ki, t)
            emit_pv(*prev)
            cx.op('dve', lambda: nc.vector.reciprocal(out=rden[:, :], in_=pden[:, :]), reads=[pdenb], writes=[rdenb])
            cx.op('dve', lambda: nc.vector.tensor_tensor(out=osb[:, :], in0=pnum[:, :], in1=rden[:, :], op=ALU.mult),
                  reads=[pnumb, rdenb], writes=[osbb])
            cx.dma('sp', S['aT'][hd * 64:(hd + 1) * 64, q0:q0 + 512], osb[:, :], reads=[osbb])
        if hd % 4 == 3 and hd != NHA - 1:
            cx.barrier()
    st.close()


def a3_stage(cx, P, G, S, x_in, x_out, L, ij=4, T=256):
    nc = cx.nc
    st = Stage(cx, 'a3')
    wo = st.sb([64, 16, D], BF16, 'wo'); wob = [st.buf(f'wo{h}') for h in range(16)]
    wstg = st.sb([64, 2, 4, D], F32, 'wstg'); wstgb = [st.buf(), st.buf()]
    for hg in range(4):
        j = hg % 2
        cx.dma('sp', wstg[:, j, :, :], P['attn_wout'][hg * 256:(hg + 1) * 256, :].rearrange("(h p) d -> p h d", p=64),
               writes=[wstgb[j]])
        cx.op('act', lambda hg=hg, j=j: nc.scalar.copy(out=wo[:, hg * 4:(hg + 1) * 4, :], in_=wstg[:, j, :, :]),
              reads=[wstgb[j]], writes=wob[hg * 4:(hg + 1) * 4])
    av = S['aT'].rearrange("(h p) t -> p h t", p=64)
    a_f = [st.sb([64, 16, T], F32, f'af{i}') for i in range(2)]; afb = [st.buf(), st.buf()]
    a_b = st.sb([64, 16, T], BF16, 'ab'); abb = st.buf('ab')
    xs = [st.sb([128, 8, T], F32, f'x{i}') for i in range(2)]; xb = [st.buf(), st.buf()]
    po = [st.ps([128, T], F32, f'po{i}') for i in range(2)]; pob = [st.buf(), st.buf()]
    GT = G['GT'][:, ij, :]
    for i in range(L // T):
        s = i % 2
        sl = slice(i * T, (i + 1) * T)
        cx.dma('sp', a_f[s][:, :, :], av[:, :, sl], writes=[afb[s]])
        cx.dma('sp', xs[s][:, :, :], x_in[:, :, sl], writes=[xb[s]])
        cx.op('act', lambda: nc.scalar.copy(out=a_b[:, :, :], in_=a_f[s][:, :, :]), reads=[afb[s]], writes=[abb])
        for dc in range(8):
            j = dc % 2
            for hd in range(16):
                cx.op('pe', lambda hd=hd, dc=dc, j=j: nc.tensor.matmul(
                    po[j][:, :], wo[:, hd, dc * 128:(dc + 1) * 128], a_b[:, hd, :], start=(hd == 0), stop=(hd == 15)),
                    reads=[wob[hd], abb], writes=[pob[j]])
            cx.op('dve', lambda dc=dc, j=j: nc.vector.scalar_tensor_tensor(
                out=xs[s][:, dc, :], in0=po[j][:, :], scalar=GT[:, dc:dc + 1], in1=xs[s][:, dc, :], op0=ALU.mult, op1=ALU.add),
                reads=[pob[j], xb[s], G['gb']], writes=[xb[s]])
        cx.dma('sp', x_out[:, :, sl], xs[s][:, :, :], reads=[xb[s]])
    st.close()
```

```python
import bisect
from contextlib import ExitStack
import numpy as np
import concourse.bass as bass
import concourse.mybir as mybir
from concourse.bass_utils import run_bass_kernel_spmd

F32, BF16 = mybir.dt.float32, mybir.dt.bfloat16
AF = mybir.ActivationFunctionType
ALU = mybir.AluOpType
AX = mybir.AxisListType

D = 1024
DFF = 2816
NFC = DFF // 128
EPS = 1e-6
ENG = ['pe', 'act', 'dve', 'pool', 'sp']


class Buf:
    __slots__ = ('w', 'r', 'name', 'dsem')

    def __init__(self, name=''):
        self.w = None
        self.r = {}
        self.name = name
        self.dsem = None


class DSem:
    __slots__ = ('h', 'n')

    def __init__(self, h):
        self.h = h
        self.n = 0


class Ctx:
    def __init__(self, nc):
        self.nc = nc
        self.eng = {'pe': nc.tensor, 'act': nc.scalar, 'dve': nc.vector, 'pool': nc.gpsimd, 'sp': nc.sync}
        self.inst = {e: [] for e in ENG}
        self.ms_seq = {e: [] for e in ENG}
        self.sem = {}
        self.nsem = 0
        for e in ('pe', 'act', 'dve'):
            self.sem[e] = nc.alloc_semaphore(f'es_{e}_{self.nsem}')
        self.known = {e: {} for e in ENG}
        self.base = {e: 0 for e in ENG}
        self.dpool = []
        self.dall = []
        self.ndma = 0

    def _resolve(self, tok):
        if tok[0] == 'd':
            return tok[1].h, id(tok[1]), tok[2]
        _, e, seq = tok
        seqs = self.ms_seq[e]
        i = bisect.bisect_left(seqs, seq)
        if i < len(seqs):
            return self.sem[e], ('e', e), i + 1
        ins = self.inst[e][seq - 1]
        ins.then_inc(self.sem[e], 1)
        seqs.append(seq)
        return self.sem[e], ('e', e), len(seqs)

    def _wait(self, e, tok):
        if tok is None:
            return
        if tok[0] == 'e' and tok[2] <= self.base[tok[1]]:
            return
        if tok[0] == 'e' and tok[1] == e:
            if e == 'pe':
                return
        h, key, val = self._resolve(tok)
        if self.known[e].get(key, 0) >= val:
            return
        self.known[e][key] = val
        self.eng[e].wait_ge(h, val)

    def _hazards(self, e, reads, writes):
        for b in reads:
            self._wait(e, b.w)
        for b in writes:
            self._wait(e, b.w)
            for t in b.r.values():
                self._wait(e, t)

    def op(self, e, fn, reads=(), writes=()):
        self._hazards(e, reads, writes)
        ins = fn()
        self.inst[e].append(ins)
        tok = ('e', e, len(self.inst[e]))
        for b in reads:
            b.r[e] = tok
        for b in writes:
            b.w = tok
            b.r = {}
        return ins

    def _get_dsem(self, b):
        if b.dsem is None:
            if self.dpool:
                b.dsem = self.dpool.pop()
            else:
                b.dsem = DSem(self.nc.alloc_semaphore(f'ds_{len(self.dall)}'))
                self.dall.append(b.dsem)
        return b.dsem

    def dma(self, q, out, in_, reads=(), writes=(), **kw):
        self._hazards(q, reads, writes)
        ins = self.eng[q].dma_start(out=out, in_=in_, **kw)
        b = writes[0] if writes else reads[0]
        ds = self._get_dsem(b)
        ins.then_inc(ds.h, 16)
        ds.n += 16
        tok = ('d', ds, ds.n)
        self.ndma += 1
        for b in reads:
            b.r[('d', id(ds))] = tok
        for b in writes:
            b.w = tok
            b.r = {}
        return ins

    def barrier(self, bufs_to_release=()):
        for e in ENG:
            for o in ('pe', 'act', 'dve'):
                if o != e and self.inst[o]:
                    self._wait(e, ('e', o, len(self.inst[o])))
            for ds in self.dall:
                if ds.n:
                    self._wait(e, ('d', ds, ds.n))
        for b in bufs_to_release:
            if b.dsem is not None:
                self.dpool.append(b.dsem)
                b.dsem = None
        self.nsem += 1
        for e in ('pe', 'act', 'dve'):
            self.base[e] = len(self.inst[e])
            if self.ms_seq[e]:
                self.sem[e] = self.nc.alloc_semaphore(f'es_{e}_{self.nsem}')
                self.ms_seq[e] = []
            for k in ENG:
                self.known[k].pop(('e', e), None)


class PView:
    def __init__(self, t, shape):
        self.t = t
        self.shape = shape
        n = 1
        for d in shape[1:]:
            n *= d
        base = t[0:shape[0], 0:n]
        if len(shape) == 3:
            base = base.rearrange("p (a b) -> p a b", a=shape[1])
        self.base = base

    def __getitem__(self, idx):
        return self.base[idx]


class Stage:
    def __init__(self, cx, name):
        self.cx = cx
        self.nc = cx.nc
        self.name = name
        self.es = ExitStack()
        self.bufs = []
        self.n = 0

    def sb(self, shape, dt, name=None):
        self.n += 1
        return self.es.enter_context(self.nc.sbuf_tensor(f'{self.name}_{name or "t"}{self.n}', list(shape), dt))

    def ps(self, shape, dt=F32, name=None):
        self.n += 1
        full = 512 if dt == F32 else 1024
        t = self.es.enter_context(self.nc.psum_tensor(f'{self.name}_{name or "p"}{self.n}', [128, full], dt))
        return PView(t, list(shape))

    def buf(self, name=''):
        b = Buf(name)
        self.bufs.append(b)
        return b

    def close(self):
        self.cx.barrier(self.bufs)
        self.es.close()


def load_w_bf16(cx, st, w_d, K, Fdim, f0=0, fw=None, name='w', stg=None):
    nc = cx.nc
    fw = fw or Fdim
    kc_n = K // 128
    t = st.sb([128, kc_n, fw], BF16, name)
    if stg is None:
        stg = (st.sb([128, 2, fw], F32, name + 'stg'), [st.buf(name + 's0'), st.buf(name + 's1')])
    stg, stgb = stg
    bufs = []
    for kc in range(kc_n):
        b = st.buf(f'{name}{kc}')
        j = kc % 2
        cx.dma('sp', stg[:, j, 0:fw], w_d[kc * 128:(kc + 1) * 128, f0:f0 + fw], writes=[stgb[j]])
        for a0 in range(0, fw, 2048):
            a1 = min(fw, a0 + 2048)
            cx.op('act', lambda kc=kc, j=j, a0=a0, a1=a1: nc.scalar.copy(out=t[:, kc, a0:a1], in_=stg[:, j, a0:a1]),
                  reads=[stgb[j]], writes=[b])
        bufs.append(b)
    return t, bufs


def load_cast_small(cx, st, dst, src_d, shape, b, name):
    nc = cx.nc
    tmp = st.sb(shape, F32, name + 'f')
    tb = st.buf(name + 'f')
    idx = tuple(slice(None) for _ in shape)
    cx.dma('sp', tmp[idx], src_d, writes=[tb])
    cx.op('dve', lambda: nc.vector.tensor_copy(out=dst[idx], in_=tmp[idx]), reads=[tb], writes=[b])


def rms_mod(cx, st, R, xt, xb, A, SH, T, nchunk=8, dim=D):
    nc = cx.nc
    sq, sqb, pss, pssb, rstd, rstdb, tmp, tmpb, h, hb = (R[k] for k in
                                                         ('sq', 'sqb', 'pss', 'pssb', 'rstd', 'rstdb', 'tmp', 'tmpb', 'h', 'hb'))
    ones = R['ones_bf']
    cx.op('act', lambda: nc.scalar.activation(out=sq[:, :, :], in_=xt, func=AF.Square), reads=[xb], writes=[sqb])
    for c in range(nchunk):
        cx.op('pe', lambda c=c: nc.tensor.matmul(pss[:, :], ones[:, :], sq[:, c, :], start=(c == 0), stop=(c == nchunk - 1)),
              reads=[sqb], writes=[pssb])
    cx.op('act', lambda: nc.scalar.activation(out=rstd[:, :], in_=pss[:, :], func=AF.Sqrt, bias=R['epsc'][:, 0:1],
                                              scale=1.0), reads=[pssb], writes=[rstdb])
    cx.op('dve', lambda: nc.vector.reciprocal(out=rstd[:, :], in_=rstd[:, :]), reads=[rstdb], writes=[rstdb])
    for c in range(nchunk):
        j = c % 2
        cx.op('dve', lambda c=c, j=j: nc.vector.scalar_tensor_tensor(out=tmp[:, j, :], in0=xt[:, c, :], scalar=float(dim) ** 0.5,
                                                                     in1=rstd[:, :], op0=ALU.mult, op1=ALU.mult),
              reads=[xb, rstdb], writes=[tmpb[j]])
        cx.op('act', lambda c=c, j=j: nc.scalar.activation(out=h[:, c, :], in_=tmp[:, j, :], func=AF.Identity,
                                                           bias=SH[:, c:c + 1], scale=A[:, c:c + 1]),
              reads=[tmpb[j]], writes=[hb])


def norm_res(st, T, nchunk=8):
    R = {}
    R['sq'] = st.sb([128, nchunk, T], BF16, 'sq'); R['sqb'] = st.buf('sq')
    R['pss'] = st.ps([128, T], F32, 'pss'); R['pssb'] = st.buf('pss')
    R['rstd'] = st.sb([128, T], F32, 'rstd'); R['rstdb'] = st.buf('rstd')
    R['tmp'] = st.sb([128, 2, T], F32, 'tmp'); R['tmpb'] = [st.buf('tmp0'), st.buf('tmp1')]
    R['h'] = st.sb([128, nchunk, T], BF16, 'h'); R['hb'] = st.buf('h')
    return R


def mod_stage(cx, P, G):
    nc = cx.nc
    st = Stage(cx, 'mod')
    ccol = st.sb([128, 8], F32, 'ccol'); ccb = st.buf('cc')
    cact = st.sb([128, 8], F32, 'cact'); cab = st.buf('ca')
    sig = st.sb([128, 8], F32, 'sig')
    mb = st.sb([128, 6, 24], F32, 'mb'); mbb = st.buf('mb')
    ng = st.sb([128, 6, 8], F32, 'ng'); ngb = st.buf('ng')
    m = st.sb([128, 6, 24], F32, 'm'); mbuf = st.buf('m')
    FW = 1536
    wt = [st.sb([128, 8, FW], F32, f'w{i}') for i in range(2)]
    wtb = [[st.buf(f'w{i}_{kc}') for kc in range(8)] for i in range(2)]
    ps = [st.ps([128, 12], F32, f'ps{i}') for i in range(2)]
    psb = [st.buf(f'ps{i}') for i in range(2)]
    cx.dma('sp', ccol[:, :], P['ccol'], writes=[ccb])
    cx.dma('sp', mb[:, :, :], P['modb'], writes=[mbb])
    cx.dma('sp', ng[:, :, :], P['ng'], writes=[ngb])
    cx.op('act', lambda: nc.scalar.activation(out=sig[:, :], in_=ccol[:, :], func=AF.Sigmoid), reads=[ccb], writes=[cab])
    cx.op('dve', lambda: nc.vector.tensor_tensor(out=cact[:, :], in0=sig[:, :], in1=ccol[:, :], op=ALU.mult),
          reads=[cab, ccb], writes=[cab])
    it = 0
    for ij in range(6):
        for half in range(2):
            s = it % 2
            it += 1
            for kc in range(8):
                cx.dma('sp', wt[s][:, kc, :], P['modw'][ij, kc * 128:(kc + 1) * 128, half * FW:(half + 1) * FW],
                       writes=[wtb[s][kc]])
            for fc in range(12):
                for kc in range(8):
                    cx.op('pe', lambda s=s, fc=fc, kc=kc: nc.tensor.matmul(
                        ps[s][:, fc:fc + 1], wt[s][:, kc, fc * 128:(fc + 1) * 128], cact[:, kc:kc + 1],
                        start=(kc == 0), stop=(kc == 7)), reads=[wtb[s][kc], cab], writes=[psb[s]])
            cx.op('dve', lambda s=s, ij=ij, half=half: nc.vector.tensor_tensor(
                out=m[:, ij, half * 12:(half + 1) * 12], in0=ps[s][:, :], in1=mb[:, ij, half * 12:(half + 1) * 12],
                op=ALU.add), reads=[psb[s], mbb], writes=[mbuf])
    A, SH, GT, HG = G['A'], G['SH'], G['GT'], G['HG']
    gb = G['gb']
    cx.op('dve', lambda: nc.vector.scalar_tensor_tensor(out=A[:, :, :], in0=m[:, :, 8:16], scalar=1.0, in1=ng[:, :, :],
                                                       op0=ALU.add, op1=ALU.mult), reads=[mbuf, ngb], writes=[gb])
    cx.op('dve', lambda: nc.vector.tensor_copy(out=SH[:, :, :], in_=m[:, :, 0:8]), reads=[mbuf], writes=[gb])
    cx.op('dve', lambda: nc.vector.tensor_copy(out=GT[:, :, :], in_=m[:, :, 16:24]), reads=[mbuf], writes=[gb])
    cx.op('dve', lambda: nc.vector.tensor_scalar(out=HG[:, :, :], in0=m[:, :, 16:24], scalar1=0.5, scalar2=0.0,
                                                 op0=ALU.mult, op1=ALU.add), reads=[mbuf], writes=[gb])
    st.close()


def ffn_stage(cx, P, G, x_in, x_out, fi, ij, L, T=256, final=False):
    nc = cx.nc
    st = Stage(cx, f'ffn{fi}')
    stg = (st.sb([128, 2, DFF], F32, 'wstg'), [st.buf('ws0'), st.buf('ws1')])
    wg, wgb = load_w_bf16(cx, st, P['wg'][fi], D, DFF, name='wg', stg=stg)
    wu, wub = load_w_bf16(cx, st, P['wu'][fi], D, DFF, name='wu', stg=stg)
    wd, wdb = load_w_bf16(cx, st, P['wd'][fi], DFF, D, name='wd', stg=stg)
    R = norm_res(st, T)
    R['ones_bf'] = G['ones_bf']; R['epsc'] = G['epsc']
    xs = [st.sb([128, 8, T], F32, f'x{i}') for i in range(2)]
    xb = [st.buf(f'x{i}') for i in range(2)]
    act = st.sb([128, NFC, T], BF16, 'act'); actb = [st.buf(f'act{f}') for f in range(NFC)]
    sg = st.sb([128, 2, T], F32, 'sg'); sgb = [st.buf('sg0'), st.buf('sg1')]
    psg = [st.ps([128, T], F32, f'pg{i}') for i in range(2)]; pgb = [st.buf(), st.buf()]
    psu = [st.ps([128, T], F32, f'pu{i}') for i in range(2)]; pub = [st.buf(), st.buf()]
    pso = [st.ps([128, T], F32, f'po{i}') for i in range(2)]; pob = [st.buf(), st.buf()]
    A, SH, HG = G['A'][:, ij, :], G['SH'][:, ij, :], G['HG'][:, ij, :]
    gb = G['gb']
    h, hb = R['h'], R['hb']
    if final:
        fng = st.sb([128, 8], F32, 'fng'); fngb = st.buf('fng')
        cx.dma('sp', fng[:, :], P['fng'], writes=[fngb])
    for i in range(L // T):
        s = i % 2
        xt = xs[s]
        cx.dma('sp', xt[:, :, :], x_in[:, :, i * T:(i + 1) * T], writes=[xb[s]])
        rms_mod(cx, st, R, xt[:, :, :], xb[s], A, SH, T)
        for fc in range(NFC):
            j = fc % 2
            for kc in range(8):
                cx.op('pe', lambda kc=kc, fc=fc, j=j: nc.tensor.matmul(
                    psg[j][:, :], wg[:, kc, fc * 128:(fc + 1) * 128], h[:, kc, :], start=(kc == 0), stop=(kc == 7)),
                    reads=[wgb[kc], hb], writes=[pgb[j]])
            for kc in range(8):
                cx.op('pe', lambda kc=kc, fc=fc, j=j: nc.tensor.matmul(
                    psu[j][:, :], wu[:, kc, fc * 128:(fc + 1) * 128], h[:, kc, :], start=(kc == 0), stop=(kc == 7)),
                    reads=[wub[kc], hb], writes=[pub[j]])
            cx.op('act', lambda j=j: nc.scalar.activation(out=sg[:, j, :], in_=psg[j][:, :], func=AF.Silu),
                  reads=[pgb[j]], writes=[sgb[j]])
            cx.op('dve', lambda j=j, fc=fc: nc.vector.tensor_tensor(out=act[:, fc, :], in0=psu[j][:, :], in1=sg[:, j, :],
                                                                    op=ALU.mult), reads=[pub[j], sgb[j]], writes=[actb[fc]])
        for dc in range(8):
            j = dc % 2
            for fc in range(NFC):
                cx.op('pe', lambda fc=fc, dc=dc, j=j: nc.tensor.matmul(
                    pso[j][:, :], wd[:, fc, dc * 128:(dc + 1) * 128], act[:, fc, :], start=(fc == 0), stop=(fc == NFC - 1)),
                    reads=[wdb[fc], actb[fc]], writes=[pob[j]])
            cx.op('dve', lambda dc=dc, j=j, xt=xt: nc.vector.scalar_tensor_tensor(
                out=xt[:, dc, :], in0=pso[j][:, :], scalar=HG[:, dc:dc + 1], in1=xt[:, dc, :], op0=ALU.mult, op1=ALU.add),
                reads=[pob[j], xb[s], gb], writes=[xb[s]])
        if final:
            final_norm(cx, st, R, xt, xb[s], fng, fngb, T)
        cx.dma('sp', x_out[:, :, i * T:(i + 1) * T], xt[:, :, :], reads=[xb[s]])
    st.close()


def final_norm(cx, st, R, xt, xb, fng, fngb, T):
    nc = cx.nc
    sq, sqb, pss, pssb, rstd, rstdb = (R[k] for k in ('sq', 'sqb', 'pss', 'pssb', 'rstd', 'rstdb'))
    ones = R['ones_bf']
    cx.op('act', lambda: nc.scalar.activation(out=sq[:, :, :], in_=xt[:, :, :], func=AF.Square), reads=[xb], writes=[sqb])
    for c in range(8):
        cx.op('pe', lambda c=c: nc.tensor.matmul(pss[:, :], ones[:, :], sq[:, c, :], start=(c == 0), stop=(c == 7)),
              reads=[sqb], writes=[pssb])
    cx.op('act', lambda: nc.scalar.activation(out=rstd[:, :], in_=pss[:, :], func=AF.Sqrt, bias=R['epsc'][:, 0:1],
                                              scale=1.0), reads=[pssb], writes=[rstdb])
    cx.op('dve', lambda: nc.vector.reciprocal(out=rstd[:, :], in_=rstd[:, :]), reads=[rstdb], writes=[rstdb])
    cx.op('dve', lambda: nc.vector.tensor_scalar(out=rstd[:, :], in0=rstd[:, :], scalar1=float(D) ** 0.5, scalar2=0.0,
                                                 op0=ALU.mult, op1=ALU.add), reads=[rstdb], writes=[rstdb])
    for c in range(8):
        cx.op('dve', lambda c=c: nc.vector.scalar_tensor_tensor(
            out=xt[:, c, :], in0=xt[:, c, :], scalar=fng[:, c:c + 1], in1=rstd[:, :], op0=ALU.mult, op1=ALU.mult),
            reads=[xb, rstdb, fngb], writes=[xb])


def build(L, plan, dbg=()):
    nc = bass.Bass("TRN2", target_bir_lowering=False)
    P = {}

    def din(name, shape, dt=F32):
        P[name] = nc.dram_tensor(name, list(shape), dt, kind="ExternalInput").ap()

    din('xT', [128, 8, L]); din('ccol', [128, 8]); din('modw', [6, D, 3 * D]); din('modb', [128, 6, 24])
    din('ng', [128, 6, 8]); din('fng', [128, 8])
    din('wg', [4, D, DFF]); din('wu', [4, D, DFF]); din('wd', [4, DFF, D])
    din('ssm_win', [D, SSM_IN]); din('convw', [128, 32, 4]); din('convb', [128, 32]); din('dtbias', [128, 32])
    din('alog', [128, 32]); din('dskip', [128, 32]); din('ssm_nw', [64, 32]); din('ssm_wout', [2048, D])
    din('ident', [128, 128]); din('identf', [128, 128]); din('triu', [128, 128])
    din('attn_wqkv', [D, 3 * D]); din('attn_wout', [D, D])
    din('EE', [64, 32, 128]); din('CM', [128, 4, 512]); din('abase', [128, 67]); din('qlc', [128, 4])
    S = {}
    zs_d = nc.dram_tensor('zs', [2048, L], F32).ap()
    S['zs'] = zs_d.rearrange("(c q) t -> q c t", q=128); S['zs64'] = zs_d.rearrange("(h p) t -> p h t", p=64)
    S['bct'] = nc.dram_tensor('bct', [2048, L], BF16).ap().rearrange("(c q) t -> q c t", q=128)
    S['xtok'] = nc.dram_tensor('xtok', [L, 3072], BF16).ap()
    S['dtt'] = nc.dram_tensor('dtt', [L, 32], F32).ap()
    if 'yT' in dbg:
        yT_d = nc.dram_tensor('yT', [2048, L], F32, kind="ExternalOutput").ap()
    else:
        yT_d = nc.dram_tensor('yT', [2048, L], F32).ap()
    S['yT'] = yT_d.rearrange("(h p) t -> p h t", p=64)
    kw_ = dict(kind="ExternalOutput") if 'dump' in dbg else {}
    if 'dump' in dbg:
        S['dbgsel'] = nc.dram_tensor('dbgsel', [L, 65], F32, kind="ExternalOutput").ap()
        S['dbgmb'] = nc.dram_tensor('dbgmb', [64, L], BF16, kind="ExternalOutput").ap()
    S['qT'] = nc.dram_tensor('qT', [D, L], F32, **kw_).ap()
    S['kT'] = nc.dram_tensor('kT', [D, L], BF16, **kw_).ap()
    S['vtok'] = nc.dram_tensor('vtok', [L, D], BF16, **kw_).ap()
    S['kmT'] = nc.dram_tensor('kmT', [D, 32], F32, **kw_).ap()
    if 'aT' in dbg:
        S['aT'] = nc.dram_tensor('aT', [D, L], F32, kind="ExternalOutput").ap()
    else:
        S['aT'] = nc.dram_tensor('aT', [D, L], F32).ap()
    P['out'] = nc.dram_tensor('outT', [128, 8, L], F32, kind="ExternalOutput").ap()
    xa = nc.dram_tensor('xa', [128, 8, L], F32).ap()
    xb_ = nc.dram_tensor('xb', [128, 8, L], F32).ap()
    cx = Ctx(nc)
    with ExitStack() as es:
        G = {}
        for k in ('A', 'SH', 'GT', 'HG'):
            G[k] = es.enter_context(nc.sbuf_tensor(f'g_{k}', [128, 6, 8], F32))
        G['gb'] = Buf('g')
        G['ones_bf'] = es.enter_context(nc.sbuf_tensor('g_ones', [128, 128], BF16))
        es.enter_context(nc.Block())
        ob = Buf('ones')
        cx.op('dve', lambda: nc.vector.memset(G['ones_bf'][:, :], 1.0), writes=[ob])
        G['epsc'] = es.enter_context(nc.sbuf_tensor('g_eps', [128, 2], F32))
        G['onec'] = es.enter_context(nc.sbuf_tensor('g_one', [128, 1], F32))
        cx.op('dve', lambda: nc.vector.memset(G['onec'][:, :], 1.0), writes=[ob])
        cx.op('dve', lambda: nc.vector.memset(G['epsc'][:, 0:1], float(D) * EPS), writes=[ob])
        cx.op('dve', lambda: nc.vector.memset(G['epsc'][:, 1:2], 2048.0 * EPS), writes=[ob])
        cx.barrier()
        cur = P['xT']
        pp = [xa, xb_]
        npp = 0
        for si, s in enumerate(plan):
            last = (si == len(plan) - 1)
            if s == 'mod':
                mod_stage(cx, P, G)
            elif s == 'mamba':
                dst = P['out'] if last else pp[npp % 2]
                m1_stage(cx, P, G, S, cur, L, T=128)
                m2_stage(cx, P, G, S, L)
                m3_stage(cx, P, G, S, cur, dst, L, T=128)
                cur = dst
                npp += 1
            elif s == 'moba':
                dst = P['out'] if last else pp[npp % 2]
                a1_stage(cx, P, G, S, cur, L, dbg=dbg)
                if 'a1only' not in dbg:
                    a2_stage(cx, P, G, S, L)
                if 'noa3' not in dbg:
                    a3_stage(cx, P, G, S, cur, dst, L)
                cur = dst
                npp += 1
            elif s.startswith('ffn'):
                fi = int(s[3])
                ij = (fi // 2) * 3 + (0 if fi % 2 == 0 else 2)
                dst = P['out'] if last else pp[npp % 2]
                ffn_stage(cx, P, G, cur, dst, fi, ij, L, final=(s.endswith('F')))
                cur = dst
                npp += 1
        cx.barrier()
    return nc


def colify(v, n):
    return np.ascontiguousarray(np.asarray(v, np.float32).reshape(n, 128).T)


def prep_core(b, inp, L):
    x = np.asarray(inp['x'][b, :L], np.float32)
    m = {}
    m['xT'] = np.ascontiguousarray(x.T.reshape(8, 128, L).transpose(1, 0, 2))
    m['ccol'] = colify(inp['c'][b], 8)
    m['modw'] = np.ascontiguousarray(np.asarray(inp['mod_w'], np.float32).reshape(6, D, 3 * D))
    mbv = np.asarray(inp['mod_b'], np.float32).reshape(6, 24, 128)
    m['modb'] = np.ascontiguousarray(mbv.transpose(2, 0, 1))
    ngv = np.asarray(inp['norm_g'], np.float32).reshape(6, 8, 128)
    m['ng'] = np.ascontiguousarray(ngv.transpose(2, 0, 1))
    m['fng'] = colify(inp['final_norm_g'], 8)
    m['wg'] = np.ascontiguousarray(np.asarray(inp['ffn_w_gate'], np.float32).reshape(4, D, DFF))
    m['wu'] = np.ascontiguousarray(np.asarray(inp['ffn_w_up'], np.float32).reshape(4, D, DFF))
    m['wd'] = np.ascontiguousarray(np.asarray(inp['ffn_w_down'], np.float32).reshape(4, DFF, D))
    m['ssm_win'] = np.ascontiguousarray(inp['ssm_w_in'][0])
    cwv = np.asarray(inp['ssm_conv_w'], np.float32)[0, :, 0, :]
    m['convw'] = np.ascontiguousarray(cwv.reshape(4, 32, 128).transpose(2, 1, 0))
    m['convb'] = colify(inp['ssm_conv_b'][0], 32)
    bc = lambda v: np.ascontiguousarray(np.broadcast_to(np.asarray(v, np.float32).reshape(1, 32), (128, 32)))
    m['dtbias'] = bc(inp['ssm_dt_bias'][0]); m['alog'] = bc(inp['ssm_a_log'][0]); m['dskip'] = bc(inp['ssm_d'][0])
    m['ssm_nw'] = np.ascontiguousarray(np.asarray(inp['ssm_norm_w'][0], np.float32).reshape(32, 64).T)
    m['ssm_wout'] = np.ascontiguousarray(inp['ssm_w_out'][0])
    m['ident'] = np.eye(128, dtype=np.float32); m['identf'] = np.eye(128, dtype=np.float32)
    m['triu'] = np.triu(np.ones((128, 128), np.float32))
    m['attn_wqkv'] = np.ascontiguousarray(inp['attn_w_qkv'][0]); m['attn_wout'] = np.ascontiguousarray(inp['attn_w_out'][0])
    m.update(attn_consts())
    return m


def attn_consts():
    EE = np.zeros((64, 32, 128), np.float32)
    for j in range(32):
        EE[j, j, :] = 1.0
        EE[32 + j, j, :] = 1.0
    CM = np.zeros((128, 4, 512), np.float32)
    p = np.arange(128)[:, None]
    ql = np.arange(256)[None, :]
    for kk in range(2):
        m_ = np.where(kk * 128 + p > ql, NEG, 0.0).astype(np.float32)
        CM[:, kk, 0:256] = m_
        CM[:, 2 + kk, 256:512] = m_
    abase = (np.arange(128, dtype=np.float32)[:, None] - 128.0 * (np.arange(67, dtype=np.float32)[None, :] - 3.0))
    qlc = (np.arange(4, dtype=np.float32)[None, :] * 128.0 + np.arange(128, dtype=np.float32)[:, None])
    return {'EE': EE, 'CM': CM, 'abase': np.ascontiguousarray(abase.astype(np.float32)),
            'qlc': np.ascontiguousarray(qlc.astype(np.float32))}


def unT(o, L):
    return np.ascontiguousarray(o.transpose(1, 0, 2).reshape(D, L).T)


NH = 32
SSM_IN = 6176


def m1_stage(cx, P, G, S, x_in, L, ij=1, T=256):
    nc = cx.nc
    st = Stage(cx, 'm1')
    win, winb = load_w_bf16(cx, st, P['ssm_win'], D, SSM_IN, name='win')
    R = norm_res(st, T); R['ones_bf'] = G['ones_bf']; R['epsc'] = G['epsc']
    h, hb = R['h'], R['hb']
    xs = [st.sb([128, 8, T], F32, f'x{i}') for i in range(2)]; xb = [st.buf(), st.buf()]
    cw = st.sb([128, 32, 4], F32, 'cw'); cb = st.sb([128, 32], F32, 'cb'); cwb = st.buf('cw')
    dtb = st.sb([128, 32], F32, 'dtb'); dtbb = st.buf('dtb')
    identb = st.sb([128, 128], BF16, 'identb'); idb = st.buf('id')
    cx.dma('sp', cw[:, :, :], P['convw'], writes=[cwb])
    cx.dma('sp', cb[:, :], P['convb'], writes=[cwb])
    cx.dma('sp', dtb[:, :], P['dtbias'], writes=[dtbb])
    load_cast_small(cx, st, identb, P['ident'], [128, 128], idb, 'idc')
    raw = st.sb([128, 32, T + 3], F32, 'raw'); rawb = [st.buf(f'raw{c}') for c in range(32)]
    for c in range(32):
        cx.op('dve', lambda c=c: nc.vector.memset(raw[:, c, 0:3], 0.0), writes=[rawb[c]])
    acc = st.sb([128, 4, T], F32, 'acc'); accb = [st.buf() for _ in range(4)]
    cv = st.sb([128, 32, T], BF16, 'cv'); cvb = [st.buf(f'cv{c}') for c in range(32)]
    zt = st.sb([128, 16, T], F32, 'zt'); ztb = st.buf('zt')
    tok = st.sb([128, T // 128, 3072], BF16, 'tok'); tokb = st.buf('tok')
    dtt = st.sb([128, T // 128, 32], F32, 'dtt'); dttb = st.buf('dtt')
    pm = [st.ps([128, T], F32, f'pm{i}') for i in range(2)]; pmb = [st.buf(), st.buf()]
    pt = [st.ps([128, 512], BF16, f'pt{i}') for i in range(2)]; ptb = [st.buf(), st.buf()]
    pd = st.ps([128, 32], F32, 'pd'); pdb = st.buf('pd')
    A, SH = G['A'][:, ij, :], G['SH'][:, ij, :]
    nmm = 0
    for i in range(L // T):
        s = i % 2
        xt = xs[s]
        cx.dma('sp', xt[:, :, :], x_in[:, :, i * T:(i + 1) * T], writes=[xb[s]])
        rms_mod(cx, st, R, xt[:, :, :], xb[s], A, SH, T)
        def mm_chunk(oc):
            nonlocal nmm
            j = nmm % 2
            nmm += 1
            for kc in range(8):
                cx.op('pe', lambda kc=kc: nc.tensor.matmul(
                    pm[j][:, :], win[:, kc, oc * 128:(oc + 1) * 128], h[:, kc, :], start=(kc == 0), stop=(kc == 7)),
                    reads=[winb[kc], hb], writes=[pmb[j]])
            return j

        for oc in range(16):
            j = mm_chunk(oc)
            cx.op('act', lambda oc=oc, j=j: nc.scalar.activation(out=zt[:, oc, :], in_=pm[j][:, :], func=AF.Silu),
                  reads=[pmb[j]], writes=[ztb])
        for c0 in range(0, 32, 4):
            pair = (c0, c0 + 1, c0 + 2, c0 + 3)
            for c in pair:
                j = mm_chunk(16 + c)
                cx.op('act', lambda c=c, j=j: nc.scalar.copy(out=raw[:, c, 3:T + 3], in_=pm[j][:, :]),
                      reads=[pmb[j]], writes=[rawb[c]])
            for c in pair:
                a = c % 4
                cx.op('dve', lambda c=c, a=a: nc.vector.tensor_scalar(
                    out=acc[:, a, :], in0=raw[:, c, 3:T + 3], scalar1=cw[:, c, 3:4], scalar2=cb[:, c:c + 1],
                    op0=ALU.mult, op1=ALU.add), reads=[rawb[c], cwb], writes=[accb[a]])
            for k in range(3):
                for c in pair:
                    a = c % 4
                    cx.op('dve', lambda c=c, a=a, k=k: nc.vector.scalar_tensor_tensor(
                        out=acc[:, a, :], in0=raw[:, c, k:k + T], scalar=cw[:, c, k:k + 1], in1=acc[:, a, :],
                        op0=ALU.mult, op1=ALU.add), reads=[rawb[c], cwb, accb[a]], writes=[accb[a]])
            for c in pair:
                a = c % 4
                cx.op('act', lambda c=c, a=a: nc.scalar.activation(out=cv[:, c, :], in_=acc[:, a, :], func=AF.Silu),
                      reads=[accb[a]], writes=[cvb[c]])
            for c in pair:
                cx.op('dve', lambda c=c: nc.vector.tensor_copy(out=raw[:, c, 0:3], in_=raw[:, c, T:T + 3]),
                      reads=[rawb[c]], writes=[rawb[c]])
        cx.dma('sp', S['zs'][:, :, i * T:(i + 1) * T], zt[:, :, :], reads=[ztb])
        cx.dma('sp', S['bct'][:, :, i * T:(i + 1) * T], cv[:, 16:32, :], reads=cvb[16:32])
        ntp = 0
        for sub in range(T // 128):
            for c4 in range(6):
                j = ntp % 2
                ntp += 1
                for q in range(4):
                    c = c4 * 4 + q
                    cx.op('pe', lambda c=c, q=q, j=j, sub=sub: nc.tensor.transpose(
                        pt[j][:, q * 128:(q + 1) * 128], cv[:, c, sub * 128:(sub + 1) * 128], identb[:, :]),
                        reads=[cvb[c], idb], writes=[ptb[j]])
                cx.op('act', lambda c4=c4, j=j, sub=sub: nc.scalar.copy(out=tok[:, sub, c4 * 512:(c4 + 1) * 512], in_=pt[j][:, :]),
                      reads=[ptb[j]], writes=[tokb])
            for kc in range(8):
                cx.op('pe', lambda kc=kc, sub=sub: nc.tensor.matmul(
                    pd[:, :], h[:, kc, sub * 128:(sub + 1) * 128], win[:, kc, 6144:6176], start=(kc == 0), stop=(kc == 7)),
                    reads=[winb[kc], hb], writes=[pdb])
            cx.op('dve', lambda sub=sub: nc.vector.tensor_tensor(out=dtt[:, sub, :], in0=pd[:, :], in1=dtb[:, :], op=ALU.add),
                  reads=[pdb, dtbb], writes=[dttb])
        cx.op('act', lambda: nc.scalar.activation(out=dtt[:, :, :], in_=dtt[:, :, :], func=AF.Exp), reads=[dttb], writes=[dttb])
        cx.op('act', lambda: nc.scalar.activation(out=dtt[:, :, :], in_=dtt[:, :, :], func=AF.Ln, bias=G['onec'][:, 0:1],
                                                  scale=1.0), reads=[dttb], writes=[dttb])
        cx.dma('sp', S['xtok'][i * T:(i + 1) * T, :].rearrange("(n p) c -> p n c", p=128), tok[:, :, :], reads=[tokb])
        cx.dma('sp', S['dtt'][i * T:(i + 1) * T, :].rearrange("(n p) c -> p n c", p=128), dtt[:, :, :], reads=[dttb])
    st.close()


def m2_stage(cx, P, G, S, L):
    nc = cx.nc
    st = Stage(cx, 'm2')
    Q = 128
    triu = st.sb([128, 128], F32, 'triu'); onesf = st.sb([128, 128], F32, 'onesf'); tri01 = st.sb([128, 128], F32, 'tri01')
    cb = st.buf('const')
    cx.dma('sp', triu[:, :], P['triu'], writes=[cb])
    cx.dma('sp', tri01[:, :], P['triu'], writes=[cb])
    cx.op('dve', lambda: nc.vector.memset(onesf[:, :], 1.0), writes=[cb])
    abc = st.sb([128, 32], F32, 'abc'); dsk = st.sb([128, 32], F32, 'dsk')
    cx.dma('sp', abc[:, :], P['alog'], writes=[cb])
    cx.dma('sp', dsk[:, :], P['dskip'], writes=[cb])
    identf = st.sb([128, 128], F32, 'identf')
    cx.dma('sp', identf[:, :], P['identf'], writes=[cb])
    cx.op('act', lambda: nc.scalar.activation(out=abc[:, :], in_=abc[:, :], func=AF.Exp), reads=[cb], writes=[cb])
    cx.op('dve', lambda: nc.vector.tensor_scalar(out=abc[:, :], in0=abc[:, :], scalar1=-1.0, scalar2=0.0, op0=ALU.mult,
                                                 op1=ALU.add), reads=[cb], writes=[cb])
    DI = st.sb([128, 32, 128], BF16, 'DI')
    cx.op('dve', lambda: nc.vector.tensor_tensor(out=DI[:, :, :], in0=identf[:, :].unsqueeze(1).to_broadcast([128, 32, 128]),
                                                 in1=dsk[:, :].unsqueeze(2).to_broadcast([128, 32, 128]), op=ALU.mult),
          reads=[cb], writes=[cb])
    St = st.sb([128, 32, 64], F32, 'St'); Sbf = st.sb([128, 32, 64], BF16, 'Sbf'); Sb = st.buf('S'); Sbb = st.buf('Sbf')
    cx.op('dve', lambda: nc.vector.memset(St[:, :, :], 0.0), writes=[Sb])
    cx.op('dve', lambda: nc.vector.memset(Sbf[:, :, :], 0.0), writes=[Sbb])
    xt = [st.sb([128, 3072], BF16, f'xt{i}') for i in range(2)]; xtb = [st.buf(), st.buf()]
    bc = [st.sb([128, 16, Q], BF16, f'bc{i}') for i in range(2)]; bcb = [st.buf(), st.buf()]
    dt = [st.sb([128, 32], F32, f'dt{i}') for i in range(2)]; dtb = [st.buf(), st.buf()]
    sm = st.sb([128, 8, 32], F32, 'sm'); smb = st.buf('sm')
    xw = st.sb([128, 32, 64], BF16, 'xw'); xwb = st.buf('xw')
    rhsA = st.sb([128, 4, 128], F32, 'rhsA'); rhsAb = st.buf('rhsA')
    E1 = st.sb([128, 4, 128], F32, 'E1'); E1b = st.buf('E1')
    cdec = st.sb([128, 4, 128], BF16, 'cdec'); cdecb = st.buf('cdec')
    pre = st.sb([128, 4, 128], F32, 'pre'); preb = st.buf('pre')
    cbm = st.sb([128, 128], F32, 'cbm'); cbmb = st.buf('cbm')
    WT = st.sb([128, 4, 128], BF16, 'WT'); WTb = st.buf('WT')
    ysb = [st.sb([64, 32, Q], F32, f'ysb{i}') for i in range(2)]; ysbb = [st.buf(), st.buf()]
    p_sm = st.ps([128, 2, 32], F32, 'psm'); p_smb = st.buf()
    p_cb = st.ps([128, 128], F32, 'pcb'); p_cbb = st.buf()
    p_ac = st.ps([128, 512], F32, 'pac'); p_acb = st.buf()
    p_y = [st.ps([64, Q], F32, f'py{i}') for i in range(2)]; p_yb = [st.buf(), st.buf()]
    p_s = [st.ps([128, 256], F32, f'pS{i}') for i in range(2)]; p_sb = [st.buf(), st.buf()]
    ny = 0
    for ci in range(L // Q):
        s = ci % 2
        t0 = ci * Q
        cx.dma('sp', xt[s][:, :], S['xtok'][t0:t0 + Q, :], writes=[xtb[s]])
        cx.dma('sp', bc[s][:, :, :], S['bct'][:, :, t0:t0 + Q], writes=[bcb[s]])
        cx.dma('sp', dt[s][:, :], S['dtt'][t0:t0 + Q, :], writes=[dtb[s]])
        dtA, lndt, bcol, wend, cdc = sm[:, 0, :], sm[:, 1, :], sm[:, 2, :], sm[:, 3, :], sm[:, 4, :]
        cx.op('dve', lambda: nc.vector.tensor_tensor(out=dtA, in0=dt[s][:, :], in1=abc[:, :], op=ALU.mult),
              reads=[dtb[s], cb], writes=[smb])
        cx.op('pe', lambda: nc.tensor.matmul(p_sm[:, 0, :], triu[:, :], dtA, start=True, stop=True), reads=[smb, cb], writes=[p_smb])
        cx.op('pe', lambda: nc.tensor.matmul(p_sm[:, 1, :], onesf[:, :], dtA, start=True, stop=True), reads=[smb, cb], writes=[p_smb])
        cx.op('act', lambda: nc.scalar.activation(out=lndt, in_=dt[s][:, :], func=AF.Ln), reads=[dtb[s]], writes=[smb])
        cx.op('dve', lambda: nc.vector.tensor_tensor(out=bcol, in0=lndt, in1=p_sm[:, 0, :], op=ALU.subtract),
              reads=[smb, p_smb], writes=[smb])
        cx.op('dve', lambda: nc.vector.tensor_tensor(out=wend, in0=bcol, in1=p_sm[:, 1, :], op=ALU.add),
              reads=[smb, p_smb], writes=[smb])
        cx.op('act', lambda: nc.scalar.activation(out=wend, in_=wend, func=AF.Exp), reads=[smb], writes=[smb])
        cx.op('act', lambda: nc.scalar.activation(out=cdc, in_=p_sm[:, 1, :], func=AF.Exp), reads=[p_smb], writes=[smb])
        cx.op('dve', lambda: nc.vector.tensor_tensor(
            out=xw[:, :, :], in0=xt[s][:, 0:2048].rearrange("p (h q) -> p h q", h=32),
            in1=wend.unsqueeze(2).to_broadcast([128, 32, 64]), op=ALU.mult), reads=[xtb[s], smb], writes=[xwb])
        ys = ysb[s]
        for g in range(8):
            BT = bc[s][:, g, :]
            CT = bc[s][:, 8 + g, :]
            cx.op('pe', lambda: nc.tensor.matmul(p_cb[:, :], BT, CT, start=True, stop=True), reads=[bcb[s]], writes=[p_cbb])
            cx.op('dve', lambda: nc.vector.tensor_tensor(out=cbm[:, :], in0=p_cb[:, :], in1=tri01[:, :], op=ALU.mult),
                  reads=[p_cbb, cb], writes=[cbmb])
            cx.op('dve', lambda g=g: nc.vector.tensor_tensor(
                out=rhsA[:, :, :], in0=triu[:, :].unsqueeze(1).to_broadcast([128, 4, 128]),
                in1=dtA[:, 4 * g:4 * g + 4].unsqueeze(2).to_broadcast([128, 4, 128]), op=ALU.mult),
                reads=[smb, cb], writes=[rhsAb])
            cx.op('pe', lambda: nc.tensor.matmul(p_ac[:, :], onesf[:, :], rhsA[:, :, :].rearrange("p h t -> p (h t)"),
                                                 start=True, stop=True), reads=[rhsAb, cb], writes=[p_acb])
            cx.op('act', lambda: nc.scalar.activation(out=E1[:, :, :].rearrange("p h t -> p (h t)"), in_=p_ac[:, :], func=AF.Exp),
                  reads=[p_acb], writes=[E1b])
            cx.op('dve', lambda: nc.vector.tensor_tensor(out=cdec[:, :, :], in0=E1[:, :, :],
                                                         in1=CT.unsqueeze(1).to_broadcast([128, 4, 128]), op=ALU.mult),
                  reads=[E1b, bcb[s]], writes=[cdecb])
            for hh in range(4):
                hd = 4 * g + hh
                cx.op('dve', lambda hh=hh, hd=hd: nc.vector.tensor_scalar(
                    out=pre[:, hh, :], in0=p_ac[:, hh * 128:(hh + 1) * 128], scalar1=bcol[:, hd:hd + 1], scalar2=20.0,
                    op0=ALU.add, op1=ALU.min), reads=[p_acb, smb], writes=[preb])
            cx.op('act', lambda: nc.scalar.activation(out=pre[:, :, :], in_=pre[:, :, :], func=AF.Exp), reads=[preb], writes=[preb])
            cx.op('dve', lambda: nc.vector.tensor_tensor(out=WT[:, :, :], in0=pre[:, :, :],
                                                         in1=cbm[:, :].unsqueeze(1).to_broadcast([128, 4, 128]), op=ALU.mult),
                  reads=[preb, cbmb], writes=[WTb])
            for hh in range(4):
                hd = 4 * g + hh
                j = ny % 2
                ny += 1
                xh = xt[s][:, hd * 64:(hd + 1) * 64]
                cx.op('pe', lambda hh=hh, j=j, xh=xh: nc.tensor.matmul(p_y[j][:, :], xh, WT[:, hh, :], start=True, stop=False),
                      reads=[xtb[s], WTb], writes=[p_yb[j]])
                cx.op('pe', lambda hd=hd, j=j, xh=xh: nc.tensor.matmul(p_y[j][:, :], xh, DI[:, hd, :], start=False, stop=False),
                      reads=[xtb[s], cb], writes=[p_yb[j]])
                cx.op('pe', lambda hh=hh, hd=hd, j=j: nc.tensor.matmul(p_y[j][:, :], Sbf[:, hd, :], cdec[:, hh, :], start=False, stop=True),
                      reads=[Sbb, cdecb], writes=[p_yb[j]])
                cx.op('act', lambda hd=hd, j=j, ys=ys: nc.scalar.copy(out=ys[:, hd, :], in_=p_y[j][:, :]),
                      reads=[p_yb[j]], writes=[ysbb[s]])
        cx.dma('sp', S['yT'][:, :, t0:t0 + Q], ys[:, :, :], reads=[ysbb[s]])
        cx.op('dve', lambda: nc.vector.tensor_tensor(out=St[:, :, :], in0=St[:, :, :],
                                                     in1=cdc.unsqueeze(2).to_broadcast([128, 32, 64]), op=ALU.mult),
              reads=[Sb, smb], writes=[Sb])
        for g in range(8):
            j = g % 2
            cx.op('pe', lambda g=g, j=j: nc.tensor.matmul(
                p_s[j][:, :], xt[s][:, 2048 + g * 128:2048 + (g + 1) * 128],
                xw[:, 4 * g:4 * g + 4, :].rearrange("p h q -> p (h q)"), start=True, stop=True),
                reads=[xtb[s], xwb], writes=[p_sb[j]])
            cx.op('dve', lambda g=g, j=j: nc.vector.tensor_tensor(
                out=St[:, 4 * g:4 * g + 4, :].rearrange("p h q -> p (h q)"),
                in0=St[:, 4 * g:4 * g + 4, :].rearrange("p h q -> p (h q)"), in1=p_s[j][:, :], op=ALU.add),
                reads=[Sb, p_sb[j]], writes=[Sb])
        cx.op('act', lambda: nc.scalar.copy(out=Sbf[:, :, :], in_=St[:, :, :]), reads=[Sb], writes=[Sbb])
    st.close()


def m3_stage(cx, P, G, S, x_in, x_out, L, ij=1, T=256):
    nc = cx.nc
    st = Stage(cx, 'm3')
    wo = st.sb([64, 32, D], BF16, 'wo'); wob = [st.buf(f'wo{h}') for h in range(32)]
    wstg = st.sb([64, 2, 4, D], F32, 'wstg'); wstgb = [st.buf(), st.buf()]
    for hgrp in range(8):
        j = hgrp % 2
        cx.dma('sp', wstg[:, j, :, :], P['ssm_wout'][hgrp * 256:(hgrp + 1) * 256, :].rearrange("(h p) d -> p h d", p=64),
               writes=[wstgb[j]])
        cx.op('act', lambda hgrp=hgrp, j=j: nc.scalar.copy(out=wo[:, hgrp * 4:(hgrp + 1) * 4, :], in_=wstg[:, j, :, :]),
              reads=[wstgb[j]], writes=wob[hgrp * 4:(hgrp + 1) * 4])
    nw = st.sb([64, 32], F32, 'nw'); nwb = st.buf('nw')
    cx.dma('sp', nw[:, :], P['ssm_nw'], writes=[nwb])
    ones64 = G['ones_bf']
    ys = [st.sb([64, 32, T], F32, f'y{i}') for i in range(2)]; ysb = [st.buf(), st.buf()]
    zs = [st.sb([64, 32, T], F32, f'z{i}') for i in range(2)]; zsb = [st.buf(), st.buf()]
    xs = [st.sb([128, 8, T], F32, f'x{i}') for i in range(2)]; xb = [st.buf(), st.buf()]
    sq = st.sb([64, 32, T], BF16, 'sq'); sqb = st.buf('sq')
    yn = st.sb([64, 32, T], BF16, 'yn'); ynb = st.buf('yn')
    rstd = st.sb([64, T], F32, 'rstd'); rstdb = st.buf('rstd')
    pss = st.ps([64, T], F32, 'pss'); pssb = st.buf()
    po = [st.ps([128, T], F32, f'po{i}') for i in range(2)]; pob = [st.buf(), st.buf()]
    GT = G['GT'][:, ij, :]
    for i in range(L // T):
        s = i % 2
        sl = slice(i * T, (i + 1) * T)
        cx.dma('sp', ys[s][:, :, :], S['yT'][:, :, sl], writes=[ysb[s]])
        cx.dma('sp', zs[s][:, :, :], S['zs'][:, :, sl].rearrange("q c t -> q c t") if False else
               S['zs64'][:, :, sl], writes=[zsb[s]])
        cx.dma('sp', xs[s][:, :, :], x_in[:, :, sl], writes=[xb[s]])
        cx.op('dve', lambda: nc.vector.tensor_tensor(out=ys[s][:, :, :], in0=ys[s][:, :, :], in1=zs[s][:, :, :], op=ALU.mult),
              reads=[ysb[s], zsb[s]], writes=[ysb[s]])
        cx.op('act', lambda: nc.scalar.activation(out=sq[:, :, :], in_=ys[s][:, :, :], func=AF.Square), reads=[ysb[s]], writes=[sqb])
        for hd in range(32):
            cx.op('pe', lambda hd=hd: nc.tensor.matmul(pss[:, :], ones64[0:64, 0:64], sq[:, hd, :], start=(hd == 0), stop=(hd == 31)),
                  reads=[sqb], writes=[pssb])
        cx.op('act', lambda: nc.scalar.activation(out=rstd[:, :], in_=pss[:, :], func=AF.Sqrt, bias=G['epsc'][0:64, 1:2],
                                                  scale=1.0), reads=[pssb], writes=[rstdb])
        cx.op('dve', lambda: nc.vector.reciprocal(out=rstd[:, :], in_=rstd[:, :]), reads=[rstdb], writes=[rstdb])
        for hd in range(32):
            cx.op('dve', lambda hd=hd: nc.vector.scalar_tensor_tensor(
                out=ys[s][:, hd, :], in0=ys[s][:, hd, :], scalar=nw[:, hd:hd + 1], in1=rstd[:, :], op0=ALU.mult, op1=ALU.mult),
                reads=[ysb[s], nwb, rstdb], writes=[ysb[s]])
        cx.op('act', lambda: nc.scalar.activation(out=yn[:, :, :], in_=ys[s][:, :, :], func=AF.Copy, scale=float(2048.0 ** 0.5)),
              reads=[ysb[s]], writes=[ynb])
        for dc in range(8):
            j = dc % 2
            for hd in range(32):
                cx.op('pe', lambda hd=hd, dc=dc, j=j: nc.tensor.matmul(
                    po[j][:, :], wo[:, hd, dc * 128:(dc + 1) * 128], yn[:, hd, :], start=(hd == 0), stop=(hd == 31)),
                    reads=[wob[hd], ynb], writes=[pob[j]])
            cx.op('dve', lambda dc=dc, j=j: nc.vector.scalar_tensor_tensor(
                out=xs[s][:, dc, :], in0=po[j][:, :], scalar=GT[:, dc:dc + 1], in1=xs[s][:, dc, :], op0=ALU.mult, op1=ALU.add),
                reads=[pob[j], xb[s], G['gb']], writes=[xb[s]])
        cx.dma('sp', x_out[:, :, sl], xs[s][:, :, :], reads=[xb[s]])
    st.close()


SEQ = 8192
PLAN = ['mod', 'ffn0', 'mamba', 'ffn1', 'ffn2', 'moba', 'ffn3F']


def kernel(**inputs):
    L = SEQ
    nc = build(L, PLAN)
    maps = [prep_core(b, inputs, L) for b in range(4)]
    in_maps = [maps[c % 4] for c in range(8)]
    res = run_bass_kernel_spmd(nc, in_maps, core_ids=list(range(8)))
    out = np.stack([unT(res.results[b]['outT'], L) for b in range(4)], axis=0)
    return out.astype(np.float32)


NHA = 16
SLOPES = [2.0 ** (-8.0 * (hh + 1) / 16.0) for hh in range(16)]
NEG = -30000.0


def a1_stage(cx, P, G, S, x_in, L, ij=4, T=256, dbg=()):
    nc = cx.nc
    st = Stage(cx, 'a1')
    wq, wqb = load_w_bf16(cx, st, P['attn_wqkv'], D, 3 * D, name='wqkv')
    R = norm_res(st, T); R['ones_bf'] = G['ones_bf']; R['epsc'] = G['epsc']
    h, hb = R['h'], R['hb']
    xs = [st.sb([128, 8, T], F32, f'x{i}') for i in range(2)]; xb = [st.buf(), st.buf()]
    qt = st.sb([128, 8, T], F32, 'qt'); qtb = st.buf('qt')
    kt = st.sb([128, 8, T], BF16, 'kt'); ktb = st.buf('kt')
    vt = st.sb([128, T // 128, 1024], BF16, 'vt'); vtb = st.buf('vt')
    NB = L // 256
    kms = st.sb([128, 8, 32], F32, 'kms'); kmsb = st.buf('kms')
    kf = st.sb([128, 8, 128], F32, 'kf'); kfb = st.buf('kf')
    cx.op('dve', lambda: nc.vector.memset(kms[:, :, :], 0.0), writes=[kmsb])
    pm = [st.ps([128, T], F32, f'pm{i}') for i in range(2)]; pmb = [st.buf(), st.buf()]
    pv = [st.ps([128, 512], F32, f'pv{i}') for i in range(2)]; pvb = [st.buf(), st.buf()]
    A, SH = G['A'][:, ij, :], G['SH'][:, ij, :]
    qT_v = S['qT'].rearrange("(c q) t -> q c t", q=128)
    kT_v = S['kT'].rearrange("(c q) t -> q c t", q=128)
    n = 0
    nv = 0
    for i in range(L // T):
        s = i % 2
        xt = xs[s]
        sl = slice(i * T, (i + 1) * T)
        cx.dma('sp', xt[:, :, :], x_in[:, :, sl], writes=[xb[s]])
        rms_mod(cx, st, R, xt[:, :, :], xb[s], A, SH, T)
        for oc in range(16):
            j = n % 2
            n += 1
            for kc in range(8):
                cx.op('pe', lambda kc=kc, oc=oc, j=j: nc.tensor.matmul(
                    pm[j][:, :], wq[:, kc, oc * 128:(oc + 1) * 128], h[:, kc, :], start=(kc == 0), stop=(kc == 7)),
                    reads=[wqb[kc], hb], writes=[pmb[j]])
            if oc < 8:
                cx.op('act', lambda oc=oc, j=j: nc.scalar.activation(out=qt[:, oc, :], in_=pm[j][:, :], func=AF.Copy, scale=0.125),
                      reads=[pmb[j]], writes=[qtb])
            else:
                c = oc - 8
                cx.op('act', lambda c=c, j=j: nc.scalar.copy(out=kt[:, c, :], in_=pm[j][:, :]), reads=[pmb[j]], writes=[ktb])
        if 'nokms' not in dbg:
            cx.op('dve', lambda: nc.vector.tensor_tensor(out=kf[:, :, :], in0=kt[:, :, 0:128], in1=kt[:, :, 128:256], op=ALU.add),
                  reads=[ktb], writes=[kfb])
            w = 64
            while w >= 1:
                cx.op('dve', lambda w=w: nc.vector.tensor_tensor(out=kf[:, :, 0:w], in0=kf[:, :, 0:w], in1=kf[:, :, w:2 * w], op=ALU.add),
                      reads=[kfb], writes=[kfb])
                w //= 2
            cx.op('dve', lambda i=i: nc.vector.tensor_copy(out=kms[:, :, i:i + 1], in_=kf[:, :, 0:1]), reads=[kfb], writes=[kmsb])
        for sub in range(T // 128 if 'nov' not in dbg else 0):
            for half in range(2):
                j = nv % 2
                nv += 1
                for kc in range(8):
                    cx.op('pe', lambda kc=kc, sub=sub, half=half, j=j: nc.tensor.matmul(
                        pv[j][:, :], h[:, kc, sub * 128:(sub + 1) * 128], wq[:, kc, 2048 + half * 512:2048 + (half + 1) * 512],
                        start=(kc == 0), stop=(kc == 7)), reads=[wqb[kc], hb], writes=[pvb[j]])
                cx.op('act', lambda sub=sub, half=half, j=j: nc.scalar.copy(out=vt[:, sub, half * 512:(half + 1) * 512], in_=pv[j][:, :]),
                      reads=[pvb[j]], writes=[vtb])
        cx.dma('sp', qT_v[:, :, sl], qt[:, :, :], reads=[qtb])
        cx.dma('sp', kT_v[:, :, sl], kt[:, :, :], reads=[ktb])
        if 'nov' not in dbg:
            cx.dma('sp', S['vtok'][sl, :].rearrange("(n p) c -> p n c", p=128), vt[:, :, :], reads=[vtb])
    if 'nokms' not in dbg:
        for c in range(8):
            cx.dma('sp', S['kmT'][c * 128:(c + 1) * 128, :], kms[:, c, :], reads=[kmsb])
    st.close()


def a2_stage(cx, P, G, S, L):
    nc = cx.nc
    st = Stage(cx, 'a2')
    NT, NQ, NB = L // 128, L // 512, L // 256
    ND = NT + 3
    cb = st.buf('const')
    EE = st.sb([64, 32, 128], BF16, 'EE'); CM = st.sb([128, 4, 512], BF16, 'CM'); identb = st.sb([128, 128], BF16, 'idb')
    load_cast_small(cx, st, EE, P['EE'], [64, 32, 128], cb, 'EEc')
    load_cast_small(cx, st, CM, P['CM'], [128, 4, 512], cb, 'CMc')
    load_cast_small(cx, st, identb, P['ident'], [128, 128], cb, 'idc')
    base = st.sb([128, ND], F32, 'base'); qlc = st.sb([128, 4], F32, 'qlc'); identf = st.sb([128, 128], F32, 'idf')
    cx.dma('sp', base[:, :], P['abase'][:, 0:ND], writes=[cb])
    cx.dma('sp', qlc[:, :], P['qlc'], writes=[cb])
    cx.dma('sp', identf[:, :], P['identf'], writes=[cb])
    thr0 = st.sb([128, 1], F32, 'thr0')
    cx.op('dve', lambda: nc.vector.memset(thr0[:, :], -1e29), writes=[cb])
    ones64 = G['ones_bf'][:, 0:64]
    kth = [st.sb([64, L], BF16, f'kth{i}') for i in range(2)]; kthb = [st.buf(), st.buf()]
    vh = [st.sb([128, NT, 64], BF16, f'vh{i}') for i in range(2)]
    vhb = [[st.buf() for _ in range(NT // 4)] for _ in range(2)]
    km = [st.sb([64, 32], F32, f'km{i}') for i in range(2)]; kmb = [st.buf(), st.buf()]
    alib = st.sb([128, ND], F32, 'alib'); alibb = st.buf('alib')
    kam = st.sb([64, 4], F32, 'kam'); kamb = st.buf('kam')
    kmx = st.sb([64, L // 2], BF16, 'kmx'); kmn = st.sb([64, L // 2], BF16, 'kmn')
    kamh = st.sb([64, 1], BF16, 'kamh'); kamhb = st.buf('kamh')
    qf = [st.sb([64, 512], F32, f'qf{i}') for i in range(2)]; qfb = [st.buf(), st.buf()]
    qb = st.sb([64, 512], BF16, 'qb'); qbb = st.buf('qb')
    qa = st.sb([64, 512], BF16, 'qa'); qab = st.buf('qa')
    gm = st.sb([128, 32], F32, 'gm'); m8 = st.sb([128, 8], F32, 'm8'); sel = st.sb([128, 32], F32, 'sel')
    sel2 = st.sb([128, 64], BF16, 'sel2'); r1 = st.sb([128, 32], F32, 'r1')
    rowc = st.sb([128, 1], F32, 'rowc')
    tb = st.buf('tk')
    sel2b = st.buf('sel2')
    MB2 = st.sb([64, 512], BF16, 'MB2'); MB2b = st.buf('MB2')
    pt = [st.sb([128, 512], BF16, f'pt{i}') for i in range(4)]; ptb = [st.buf() for _ in range(4)]
    osb = st.sb([64, 512], F32, 'osb'); osbb = st.buf('osb')
    rden = st.sb([64, 512], F32, 'rden'); rdenb = st.buf('rden')
    psc = [st.ps([128, 512], F32, f'psc{i}') for i in range(3)]; pscb = [st.buf() for _ in range(3)]
    pnum = st.ps([64, 512], F32, 'pnum'); pnumb = st.buf()
    pden = st.ps([64, 512], F32, 'pden'); pdenb = st.buf()
    pg = st.ps([128, 64], F32, 'pg'); pgb = st.buf()
    ptr = st.ps([64, 512], BF16, 'ptr'); ptrb = st.buf()
    nsc = 0
    npt = 0
    nq = 0
    for hd in range(NHA):
        s = hd % 2
        slope = SLOPES[hd]
        cx.dma('sp', kth[s][:, :], S['kT'][hd * 64:(hd + 1) * 64, :], writes=[kthb[s]])
        vsrc = S['vtok'][:, hd * 64:(hd + 1) * 64].rearrange("(t p) c -> p t c", p=128)
        for vc in range(NT // 4):
            cx.dma('sp', vh[s][:, vc * 4:(vc + 1) * 4, :], vsrc[:, vc * 4:(vc + 1) * 4, :], writes=[vhb[s][vc]])
        cx.dma('sp', km[s][:, :], S['kmT'][hd * 64:(hd + 1) * 64, :], writes=[kmb[s]])
        for fo, fop in ((kmx, ALU.max), (kmn, ALU.min)):
            w = L // 2
            cx.op('dve', lambda fo=fo, fop=fop, w=w: nc.vector.tensor_tensor(out=fo[:, 0:w], in0=kth[s][:, 0:w], in1=kth[s][:, w:2 * w], op=fop),
                  reads=[kthb[s]], writes=[kamb])
            w //= 2
            while w >= 1:
                cx.op('dve', lambda fo=fo, fop=fop, w=w: nc.vector.tensor_tensor(out=fo[:, 0:w], in0=fo[:, 0:w], in1=fo[:, w:2 * w], op=fop),
                      reads=[kamb], writes=[kamb])
                w //= 2
        cx.op('dve', lambda: nc.vector.scalar_tensor_tensor(out=kam[:, 2:3], in0=kmn[:, 0:1], scalar=-1.0, in1=kmx[:, 0:1],
                                                            op0=ALU.mult, op1=ALU.max), reads=[kamb], writes=[kamb])
        cx.op('dve', lambda: nc.vector.tensor_copy(out=kamh[:, :], in_=kam[:, 2:3]), reads=[kamb], writes=[kamhb])
        cx.op('dve', lambda: nc.vector.tensor_scalar(out=alib[:, :], in0=base[:, :], scalar1=float(slope), scalar2=0.0,
                                                     op0=ALU.mult, op1=ALU.add), reads=[cb], writes=[alibb])
        for qi in range(NQ):
            q0 = qi * 512
            s2 = nq % 2
            nq += 1
            cx.dma('sp', qf[s2][:, :], S['qT'][hd * 64:(hd + 1) * 64, q0:q0 + 512], writes=[qfb[s2]])
            cx.op('act', lambda: nc.scalar.copy(out=qb[:, :], in_=qf[s2][:, :]), reads=[qfb[s2]], writes=[qbb])
            cx.op('dve', lambda: nc.vector.scalar_tensor_tensor(out=qa[:, :], in0=qf[s2][:, :], scalar=-1.0, in1=qf[s2][:, :],
                                                                op0=ALU.mult, op1=ALU.max), reads=[qfb[s2]], writes=[qab])
            for sub in range(4):
                b = (q0 + sub * 128) // 256
                qs = slice(sub * 128, (sub + 1) * 128)
                cx.op('pe', lambda qs=qs: nc.tensor.matmul(pg[:, 0:32], qf[s2][:, qs], km[s][:, :], start=True, stop=True),
                      reads=[qfb[s2], kmb[s]], writes=[pgb])
                cx.op('pe', lambda qs=qs: nc.tensor.matmul(pg[:, 32:33], qa[:, qs], kamh[:, :], start=True, stop=True),
                      reads=[qab, kamhb], writes=[pgb])
                cx.op('dve', lambda sub=sub: nc.vector.scalar_tensor_tensor(
                    out=rowc[:, :], in0=qlc[:, sub:sub + 1], scalar=-float(slope), in1=pg[:, 32:33], op0=ALU.mult, op1=ALU.subtract),
                    reads=[cb, pgb], writes=[tb])
                cx.op('dve', lambda: nc.vector.memset(gm[:, :], -1e30), writes=[tb])
                if b > 0:
                    cx.op('dve', lambda b=b: nc.vector.tensor_copy(out=gm[:, 0:b], in_=pg[:, 0:b]), reads=[pgb], writes=[tb])
                if b >= 3:
                    cx.op('dve', lambda: nc.vector.max(out=m8[:, :], in_=gm[:, :]), reads=[tb], writes=[tb])
                    thr = m8[:, 2:3]
                else:
                    thr = thr0[:, 0:1]
                cx.op('dve', lambda thr=thr: nc.vector.tensor_scalar(out=sel[:, :], in0=gm[:, :], scalar1=thr, scalar2=-NEG,
                                                                     op0=ALU.is_ge, op1=ALU.mult), reads=[tb, cb], writes=[tb])
                cx.op('dve', lambda: nc.vector.tensor_scalar(out=sel[:, :], in0=sel[:, :], scalar1=NEG, scalar2=rowc[:, 0:1],
                                                             op0=ALU.add, op1=ALU.add), reads=[tb], writes=[tb])
                cx.op('dve', lambda b=b: nc.vector.tensor_copy(out=sel[:, b:b + 1], in_=rowc[:, :]), reads=[tb], writes=[tb])
                if 'dbgsel' in S and hd == 5:
                    r0 = q0 + sub * 128
                    cx.dma('sp', S['dbgsel'][r0:r0 + 128, 0:32], sel[:, :], reads=[tb])
                    cx.dma('sp', S['dbgsel'][r0:r0 + 128, 32:64], gm[:, :], reads=[tb])
                    cx.dma('sp', S['dbgsel'][r0:r0 + 128, 64:65], rowc[:, :], reads=[tb], allow_slow_non_contiguous=True)
                cx.op('dve', lambda: nc.vector.tensor_copy(out=sel2[:, 0:32], in_=sel[:, :]), reads=[tb, sel2b], writes=[tb, sel2b])
                cx.op('dve', lambda: nc.vector.tensor_tensor(out=r1[:, :], in0=sel[:, :], in1=sel2[:, 0:32], op=ALU.subtract),
                      reads=[tb, sel2b], writes=[tb])
                cx.op('dve', lambda: nc.vector.tensor_copy(out=sel2[:, 32:64], in_=r1[:, :]), reads=[tb, sel2b], writes=[tb, sel2b])
                cx.op('pe', lambda qs=qs: nc.tensor.transpose(ptr[:, qs], sel2[:, :], identb[:, :]), reads=[sel2b, cb], writes=[ptrb])
            cx.op('act', lambda: nc.scalar.copy(out=MB2[:, :], in_=ptr[:, :]), reads=[ptrb], writes=[MB2b])
            if 'dbgsel' in S and hd == 5:
                cx.dma('sp', S['dbgmb'][:, q0:q0 + 512], MB2[:, :], reads=[MB2b])
            nkt = 4 * qi + 4

            def emit_pv(ki, t, nkt=nkt):
                cx.op('pe', lambda: nc.tensor.matmul(pnum[:, :], vh[s][:, ki, :], pt[t][:, :], start=(ki == 0), stop=(ki == nkt - 1)),
                      reads=[vhb[s][ki // 4], ptb[t]], writes=[pnumb])
                cx.op('pe', lambda: nc.tensor.matmul(pden[:, :], ones64, pt[t][:, :], start=(ki == 0), stop=(ki == nkt - 1)),
                      reads=[ptb[t]], writes=[pdenb])

            prev = None
            for ki in range(nkt):
                j = ki // 2
                kk = ki % 2
                ddi = (q0 - ki * 128) // 128 + 3
                a = nsc % 3
                nsc += 1
                diag = j >= 2 * qi
                cx.op('pe', lambda ki=ki, a=a: nc.tensor.matmul(psc[a][:, :], kth[s][:, ki * 128:(ki + 1) * 128], qb[:, :],
                                                                 start=True, stop=False), reads=[kthb[s], qbb], writes=[pscb[a]])
                cx.op('pe', lambda j=j, a=a, diag=diag: nc.tensor.matmul(psc[a][:, :], EE[:, j, :], MB2[:, :], start=False, stop=(not diag)),
                      reads=[cb, MB2b], writes=[pscb[a]])
                if diag:
                    ci = (j - 2 * qi) * 2 + kk
                    cx.op('pe', lambda ci=ci, a=a: nc.tensor.matmul(psc[a][:, :], identb[:, :], CM[:, ci, :], start=False, stop=True),
                          reads=[cb], writes=[pscb[a]])
                if prev is not None:
                    emit_pv(*prev)
                t = npt % 4
                npt += 1
                cx.op('act', lambda a=a, t=t, ddi=ddi: nc.scalar.activation(out=pt[t][:, :], in_=psc[a][:, :], func=AF.Exp,
                                                                           bias=alib[:, ddi:ddi + 1], scale=1.0),
                      reads=[pscb[a], alibb], writes=[ptb[t]])
                prev = (ki, t)
            emit_pv(*prev)
            cx.op('dve', lambda: nc.vector.reciprocal(out=rden[:, :], in_=pden[:, :]), reads=[pdenb], writes=[rdenb])
            cx.op('dve', lambda: nc.vector.tensor_tensor(out=osb[:, :], in0=pnum[:, :], in1=rden[:, :], op=ALU.mult),
                  reads=[pnumb, rdenb], writes=[osbb])
            cx.dma('sp', S['aT'][hd * 64:(hd + 1) * 64, q0:q0 + 512], osb[:, :], reads=[osbb])
        if hd % 4 == 3 and hd != NHA - 1:
            cx.barrier()
    st.close()


def a3_stage(cx, P, G, S, x_in, x_out, L, ij=4, T=256):
    nc = cx.nc
    st = Stage(cx, 'a3')
    wo = st.sb([64, 16, D], BF16, 'wo'); wob = [st.buf(f'wo{h}') for h in range(16)]
    wstg = st.sb([64, 2, 4, D], F32, 'wstg'); wstgb = [st.buf(), st.buf()]
    for hg in range(4):
        j = hg % 2
        cx.dma('sp', wstg[:, j, :, :], P['attn_wout'][hg * 256:(hg + 1) * 256, :].rearrange("(h p) d -> p h d", p=64),
               writes=[wstgb[j]])
        cx.op('act', lambda hg=hg, j=j: nc.scalar.copy(out=wo[:, hg * 4:(hg + 1) * 4, :], in_=wstg[:, j, :, :]),
              reads=[wstgb[j]], writes=wob[hg * 4:(hg + 1) * 4])
    av = S['aT'].rearrange("(h p) t -> p h t", p=64)
    a_f = [st.sb([64, 16, T], F32, f'af{i}') for i in range(2)]; afb = [st.buf(), st.buf()]
    a_b = st.sb([64, 16, T], BF16, 'ab'); abb = st.buf('ab')
    xs = [st.sb([128, 8, T], F32, f'x{i}') for i in range(2)]; xb = [st.buf(), st.buf()]
    po = [st.ps([128, T], F32, f'po{i}') for i in range(2)]; pob = [st.buf(), st.buf()]
    GT = G['GT'][:, ij, :]
    for i in range(L // T):
        s = i % 2
        sl = slice(i * T, (i + 1) * T)
        cx.dma('sp', a_f[s][:, :, :], av[:, :, sl], writes=[afb[s]])
        cx.dma('sp', xs[s][:, :, :], x_in[:, :, sl], writes=[xb[s]])
        cx.op('act', lambda: nc.scalar.copy(out=a_b[:, :, :], in_=a_f[s][:, :, :]), reads=[afb[s]], writes=[abb])
        for dc in range(8):
            j = dc % 2
            for hd in range(16):
                cx.op('pe', lambda hd=hd, dc=dc, j=j: nc.tensor.matmul(
                    po[j][:, :], wo[:, hd, dc * 128:(dc + 1) * 128], a_b[:, hd, :], start=(hd == 0), stop=(hd == 15)),
                    reads=[wob[hd], abb], writes=[pob[j]])
            cx.op('dve', lambda dc=dc, j=j: nc.vector.scalar_tensor_tensor(
                out=xs[s][:, dc, :], in0=po[j][:, :], scalar=GT[:, dc:dc + 1], in1=xs[s][:, dc, :], op0=ALU.mult, op1=ALU.add),
                reads=[pob[j], xb[s], G['gb']], writes=[xb[s]])
        cx.dma('sp', x_out[:, :, sl], xs[s][:, :, :], reads=[xb[s]])
    st.close()
```
